# Optimizing a Trainium2 kernel written in Bass

```python
import math
import jax, jax.numpy as jnp
from jax import lax
import numpy as np

D_MODEL = 1024
BATCH = 4
SEQ = 8192
DEPTH = 4

BLOCK_Q = 128
EPS = 1e-6
MLA_HEADS = 6
MLA_Q_LORA = 384
MLA_KV_LORA = 256
MLA_NOPE = 64
MLA_ROPE = 32
MLA_V = 64
ROPE_THETA = 10000.0
DSA_HEADS = 5
DSA_DIM = 64
IDX_HEADS = 8
IDX_DIM = 64
DSA_TOPK = 256
SB_HEADS = 5
SB_DIM = 64
REL_BUCKETS = 32
REL_MAX_DIST = 128
D_FF = -(-8 * D_MODEL // (3 * 256)) * 256
PLE_DIM = 256

MIX_WIDTH = MLA_HEADS * MLA_V + DSA_HEADS * DSA_DIM + SB_HEADS * SB_DIM
IN_SPLITS = (MLA_Q_LORA, MLA_KV_LORA, MLA_ROPE,
             DSA_HEADS * DSA_DIM, DSA_DIM, DSA_DIM,
             IDX_HEADS * IDX_DIM, IDX_DIM, IDX_HEADS,
             SB_HEADS * SB_DIM, SB_HEADS * SB_DIM, SB_HEADS * SB_DIM)
D_IN = sum(IN_SPLITS)

kernel_name = "hybrid_mla_dsa_stickbreak_trunk"


def rms_norm(x, g):
    xf = x.astype(jnp.float32)
    y = xf * lax.rsqrt(jnp.mean(xf * xf, axis=-1, keepdims=True) + EPS)
    return (y * g.astype(jnp.float32)).astype(x.dtype)


def apply_rope(x, pos):
    half = x.shape[-1] // 2
    inv = ROPE_THETA ** (-jnp.arange(half, dtype=jnp.float32) / half)
    ang = pos.astype(jnp.float32)[:, None] * inv[None, :]
    cos = jnp.cos(ang)[None, :, None, :].astype(x.dtype)
    sin = jnp.sin(ang)[None, :, None, :].astype(x.dtype)
    x1, x2 = x[..., :half], x[..., half:]
    return jnp.concatenate([x1 * cos - x2 * sin, x1 * sin + x2 * cos], axis=-1)


def t5_bucket(dist):
    max_exact = REL_BUCKETS // 2
    d = jnp.maximum(dist, 1).astype(jnp.float32)
    large = max_exact + (jnp.log(d / max_exact) / math.log(REL_MAX_DIST / max_exact)
                         * (REL_BUCKETS - max_exact)).astype(jnp.int32)
    large = jnp.minimum(large, REL_BUCKETS - 1)
    return jnp.where(dist < max_exact, dist, large)


def mixer_block(q0, pos, mla_q, mla_k, mla_v, dsa_q, dsa_k, dsa_v,
                idx_q, idx_k, idx_w, sb_q, sb_k, sb_v, rel_bias, topk):
    B = mla_q.shape[0]
    sl = lambda a: lax.dynamic_slice_in_dim(a, q0, BLOCK_Q, axis=1)
    qpos = q0 + jnp.arange(BLOCK_Q, dtype=jnp.int32)
    causal = pos[None, :] <= qpos[:, None]
    strict = pos[None, :] < qpos[:, None]

    s = jnp.einsum('bqhd,bkhd->bhqk', sl(mla_q), mla_k).astype(jnp.float32) * (MLA_NOPE + MLA_ROPE) ** -0.5
    s = jnp.where(causal[None, None], s, -jnp.inf)
    pr = jax.nn.softmax(s, axis=-1).astype(mla_v.dtype)
    o_mla = jnp.einsum('bhqk,bkhd->bqhd', pr, mla_v).reshape(B, BLOCK_Q, MLA_HEADS * MLA_V)

    ilog = jnp.einsum('bqhd,bkd->bqhk', sl(idx_q), idx_k) * IDX_DIM ** -0.5
    isc = jnp.einsum('bqhk,bqh->bqk', jax.nn.relu(ilog), sl(idx_w)).astype(jnp.float32)
    isc = jnp.where(causal[None], isc, -jnp.inf)
    _, sel = lax.top_k(isc, topk)
    valid = sel <= qpos[None, :, None]
    gather = jax.vmap(lambda a, i: a[i])
    kg = gather(dsa_k, sel)
    vg = gather(dsa_v, sel)
    bias = rel_bias[t5_bucket(jnp.maximum(qpos[None, :, None] - sel, 0))]
    a = (jnp.einsum('bqhd,bqjd->bhqj', sl(dsa_q), kg).astype(jnp.float32) * DSA_DIM ** -0.5
         + jnp.transpose(bias, (0, 3, 1, 2)).astype(jnp.float32))
    a = jnp.where(valid[:, None], a, -jnp.inf)
    pa = jax.nn.softmax(a, axis=-1).astype(vg.dtype)
    o_dsa = jnp.einsum('bhqj,bqjd->bqhd', pa, vg).reshape(B, BLOCK_Q, DSA_HEADS * DSA_DIM)

    z = jnp.einsum('bqhd,bkhd->bhqk', sl(sb_q), sb_k).astype(jnp.float32) * SB_DIM ** -0.5
    m = strict[None, None]
    l1m = jnp.where(m, jax.nn.log_sigmoid(-z), 0.0)
    suffix = lax.cumsum(l1m, axis=3, reverse=True) - l1m
    w = jnp.where(m, jnp.exp(jax.nn.log_sigmoid(z) + suffix), 0.0).astype(sb_v.dtype)
    o_sb = jnp.einsum('bhqk,bkhd->bqhd', w, sb_v).reshape(B, BLOCK_Q, SB_HEADS * SB_DIM)

    return jnp.concatenate([o_mla, o_dsa, o_sb], axis=-1)


def setup_inputs(seed: int = 0) -> dict:
    key = jax.random.key(seed)
    ks = jax.random.split(key, 18)
    f32 = jnp.float32
    nrm = lambda k, shape, scale: jax.random.normal(k, shape, f32) * scale
    gain = lambda k, shape: 1.0 + 0.05 * jax.random.normal(k, shape, f32)
    return {
        "x": nrm(ks[0], (BATCH, SEQ, D_MODEL), 1.0),
        "p": nrm(ks[1], (DEPTH, BATCH, SEQ, PLE_DIM), 1.0),
        "w_in": nrm(ks[2], (DEPTH, D_MODEL, D_IN), D_MODEL ** -0.5),
        "attn_norm": gain(ks[3], (DEPTH, D_MODEL)),
        "mla_q_norm": gain(ks[4], (DEPTH, MLA_Q_LORA)),
        "mla_w_uq": nrm(ks[5], (DEPTH, MLA_Q_LORA, MLA_HEADS * (MLA_NOPE + MLA_ROPE)), MLA_Q_LORA ** -0.5),
        "mla_kv_norm": gain(ks[6], (DEPTH, MLA_KV_LORA)),
        "mla_w_ukv": nrm(ks[7], (DEPTH, MLA_KV_LORA, MLA_HEADS * (MLA_NOPE + MLA_V)), MLA_KV_LORA ** -0.5),
        "rel_bias": nrm(ks[8], (REL_BUCKETS, DSA_HEADS), 0.5),
        "w_o": nrm(ks[9], (DEPTH, MIX_WIDTH, D_MODEL), MIX_WIDTH ** -0.5),
        "ffn_norm": gain(ks[10], (DEPTH, D_MODEL)),
        "w_gate": nrm(ks[11], (DEPTH, D_MODEL, D_FF), D_MODEL ** -0.5),
        "w_up": nrm(ks[12], (DEPTH, D_MODEL, D_FF), D_MODEL ** -0.5),
        "w_down": nrm(ks[13], (DEPTH, D_FF, D_MODEL), D_FF ** -0.5),
        "ple_norm": gain(ks[14], (DEPTH, D_MODEL)),
        "w_ple_gate": nrm(ks[15], (DEPTH, D_MODEL, D_MODEL), D_MODEL ** -0.5),
        "w_ple_proj": nrm(ks[16], (DEPTH, PLE_DIM, D_MODEL), PLE_DIM ** -0.5),
        "final_norm": gain(ks[17], (D_MODEL,)),
    }


def reference(x, p, w_in, attn_norm, mla_q_norm, mla_w_uq, mla_kv_norm, mla_w_ukv,
              rel_bias, w_o, ffn_norm, w_gate, w_up, w_down, ple_norm, w_ple_gate,
              w_ple_proj, final_norm):
    B, S, _ = x.shape
    n_blk = S // BLOCK_Q
    topk = min(DSA_TOPK, S // 4)
    pos = jnp.arange(S, dtype=jnp.int32)
    offsets = [int(o) for o in np.cumsum(IN_SPLITS)[:-1]]
    starts = jnp.arange(n_blk, dtype=jnp.int32) * BLOCK_Q
    h = x
    for i in range(DEPTH):
        hn = rms_norm(h, attn_norm[i])
        proj = hn @ w_in[i]
        (c_q, c_kv, k_rope, dq, dk, dv, iq, ik, iw, sq, sk, sv) = jnp.split(proj, offsets, axis=-1)

        q = (rms_norm(c_q, mla_q_norm[i]) @ mla_w_uq[i]).reshape(B, S, MLA_HEADS, MLA_NOPE + MLA_ROPE)
        q = jnp.concatenate([q[..., :MLA_NOPE], apply_rope(q[..., MLA_NOPE:], pos)], axis=-1)
        kv = (rms_norm(c_kv, mla_kv_norm[i]) @ mla_w_ukv[i]).reshape(B, S, MLA_HEADS, MLA_NOPE + MLA_V)
        kr = apply_rope(k_rope[:, :, None, :], pos)
        mla_k = jnp.concatenate([kv[..., :MLA_NOPE],
                                 jnp.broadcast_to(kr, (B, S, MLA_HEADS, MLA_ROPE))], axis=-1)
        mla_v = kv[..., MLA_NOPE:]

        dsa_q = dq.reshape(B, S, DSA_HEADS, DSA_DIM)
        idx_q = iq.reshape(B, S, IDX_HEADS, IDX_DIM)
        idx_w = iw * IDX_HEADS ** -0.5

        sb_q = sq.reshape(B, S, SB_HEADS, SB_DIM)
        sb_k = sk.reshape(B, S, SB_HEADS, SB_DIM)
        sb_v = sv.reshape(B, S, SB_HEADS, SB_DIM)

        blocks = lax.map(lambda q0: mixer_block(q0, pos, q, mla_k, mla_v, dsa_q, dk, dv,
                                                idx_q, ik, idx_w, sb_q, sb_k, sb_v,
                                                rel_bias, topk), starts)
        mix = jnp.transpose(blocks, (1, 0, 2, 3)).reshape(B, S, MIX_WIDTH)
        h = h + mix @ w_o[i]

        hf = rms_norm(h, ffn_norm[i])
        h = h + (jax.nn.silu(hf @ w_gate[i]) * (hf @ w_up[i])) @ w_down[i]

        g = jax.nn.sigmoid(rms_norm(h, ple_norm[i]) @ w_ple_gate[i])
        h = h + g * (p[i] @ w_ple_proj[i])
    return rms_norm(h, final_norm)
```

```python
import math
import os
import numpy as np
import ml_dtypes
import concourse.bass as bass
import concourse.mybir as mybir
from concourse.bass_utils import run_bass_kernel_spmd

F32 = mybir.dt.float32
BF16 = mybir.dt.bfloat16
AF = mybir.ActivationFunctionType
ALU = mybir.AluOpType
AX = mybir.AxisListType

D = 1024
DEPTH = 4
SEQ = 8192
BATCH = 4
D_IN = 2664
D_FF = 2816
NFF = D_FF // 128
EPS = 1e-6
NIT = 16
TOPK = 256
NEG = -1.0e30

O_CQ, O_CKV, O_KR, O_DQ, O_DK, O_DV, O_IQ, O_IK, O_IW, O_SQ, O_SK, O_SV = (
    0, 384, 640, 672, 992, 1056, 1120, 1632, 1696, 1704, 2024, 2344)
M_MLA, M_DSA, M_SB = 0, 384, 704

STRICT_SAME_ENGINE = os.environ.get("STRICT", "1") == "1"


class Ins:
    __slots__ = ("eng", "fn", "deps", "inc", "sem", "val", "epoch", "isdma", "slotwait")


class Buf:
    __slots__ = ("w", "r", "rd")

    def __init__(self):
        self.w = None
        self.r = {}
        self.rd = []


class T:
    def __init__(self, t):
        self.t = t
        self.b = Buf()


class Em:
    ENG = ("pe", "act", "dve", "pool", "sp")
    BLK = {"pe": "tensor", "act": "scalar", "dve": "vector", "pool": "gpsimd", "sp": "sync"}

    def __init__(self, nc, csems, dsems):
        self.nc = nc
        self.csem = csems
        self.dsem = dsems
        self.cnt = {e: 0 for e in csems}
        self.dcount = {q: 0 for q in dsems}
        self.dlast = {q: [None] * len(dsems[q]) for q in dsems}
        self.ops = {e: [] for e in self.ENG}
        self.seen = {e: {} for e in self.ENG}
        self.epoch = 0
        self.nins = 0

    def _mk(self, eng, fn, reads, writes, isdma):
        ins = Ins()
        ins.eng, ins.fn, ins.isdma, ins.inc, ins.epoch = eng, fn, isdma, False, self.epoch
        ins.sem = ins.val = ins.slotwait = None
        deps = set()
        for b in reads:
            if b.w is not None:
                deps.add(b.w)
        for b in writes:
            if b.w is not None:
                deps.add(b.w)
            deps.update(b.r.values())
            deps.update(b.rd)
        for b in reads:
            if isdma:
                b.rd.append(ins)
            else:
                b.r[eng] = ins
        for b in writes:
            b.w = ins
            b.r = {}
            b.rd = []
        out = []
        for d in deps:
            if d is ins or d.epoch != self.epoch:
                continue
            if (not isdma) and (not d.isdma) and d.eng == eng:
                if eng == "pe" or not STRICT_SAME_ENGINE:
                    continue
            out.append(d)
        ins.deps = out
        self.ops[eng].append(ins)
        self.nins += 1
        return ins

    def op(self, eng, fn, reads=(), writes=()):
        return self._mk(eng, fn, [x.b if isinstance(x, T) else x for x in reads],
                        [x.b if isinstance(x, T) else x for x in writes], False)

    def dma(self, q, out, in_, reads=(), writes=()):
        return self._mk(q, lambda e: e.dma_start(out=out, in_=in_),
                        [x.b if isinstance(x, T) else x for x in reads],
                        [x.b if isinstance(x, T) else x for x in writes], True)

    def flush(self):
        for e in self.ENG:
            for ins in self.ops[e]:
                for d in ins.deps:
                    d.inc = True
        for e in self.ENG:
            for ins in self.ops[e]:
                if ins.isdma:
                    n = len(self.dsem[e])
                    k = self.dcount[e]
                    self.dcount[e] += 1
                    slot = k % n
                    ins.sem = self.dsem[e][slot]
                    ins.val = 16 * (k // n + 1)
                    prev = self.dlast[e][slot]
                    if prev is not None and prev.epoch == self.epoch:
                        ins.slotwait = (prev.sem, prev.val)
                    self.dlast[e][slot] = ins
                elif ins.inc:
                    self.cnt[e] += 1
                    ins.sem = self.csem[e]
                    ins.val = self.cnt[e]
        finals = []
        for q in self.dsem:
            n = len(self.dsem[q])
            for slot in range(n):
                last = self.dlast[q][slot]
                if last is not None and last.epoch == self.epoch:
                    finals.append((last.sem, last.val))
        with self.nc.Block() as block:
            for e in self.ENG:
                ops = self.ops[e]
                seen = self.seen[e]

                def body(q, ops=ops, seen=seen, e=e):
                    for ins in ops:
                        waits = {}
                        for d in ins.deps:
                            key = id(d.sem)
                            if key not in waits or waits[key][1] < d.val:
                                waits[key] = (d.sem, d.val)
                        if ins.slotwait is not None:
                            key = id(ins.slotwait[0])
                            if key not in waits or waits[key][1] < ins.slotwait[1]:
                                waits[key] = ins.slotwait
                        for key, (sem, val) in waits.items():
                            if seen.get(key, 0) < val:
                                q.wait_ge(sem, val)
                                seen[key] = val
                        r = ins.fn(q)
                        if ins.isdma:
                            r.then_inc(ins.sem, 16)
                        elif ins.inc:
                            r.then_inc(ins.sem, 1)
                    if e == "sp":
                        for sem, val in finals:
                            if seen.get(id(sem), 0) < val:
                                q.wait_ge(sem, val)
                                seen[id(sem)] = val
                if ops or e == "sp":
                    getattr(block, self.BLK[e])(body)
        self.ops = {e: [] for e in self.ENG}
        self.epoch += 1


class Ring:
    def __init__(self, tiles):
        self.tiles = [T(t) for t in tiles]
        self.i = 0

    def next(self):
        t = self.tiles[self.i % len(self.tiles)]
        self.i += 1
        return t


def t5_bucket_np(dist):
    max_exact = 16
    d = np.maximum(dist, 1).astype(np.float32)
    large = max_exact + (np.log(d / np.float32(max_exact)) / np.float32(math.log(128 / max_exact))
                         * np.float32(32 - max_exact)).astype(np.int32)
    large = np.minimum(large, 31)
    return np.where(dist < max_exact, dist, large)


def make_consts(S):
    c = {}
    half = 16
    inv = (10000.0 ** (-np.arange(half, dtype=np.float32) / half)).astype(np.float32)
    ang = np.arange(S, dtype=np.float32)[:, None] * inv[None, :]
    cos = np.cos(ang).astype(np.float32).T
    sin = np.sin(ang).astype(np.float32).T
    c["cosT"] = np.ascontiguousarray(np.concatenate([cos, cos], 0))
    c["sinT"] = np.ascontiguousarray(np.concatenate([-sin, sin], 0))
    k = np.arange(128)[:, None]
    q = np.arange(512)[None, :]
    mi = np.zeros((4, 128, 512), np.float32)
    ms = np.zeros((4, 128, 512), np.float32)
    for kb in range(4):
        mi[kb] = ((kb * 128 + k) <= q)
        ms[kb] = ((kb * 128 + k) < q)
    c["mask_incl"] = mi.astype(ml_dtypes.bfloat16)
    c["mask_strict"] = ms.astype(ml_dtypes.bfloat16)
    j = np.arange(128)
    c["tri"] = (j[:, None] >= j[None, :]).astype(np.float32).astype(ml_dtypes.bfloat16)
    c["ones"] = np.ones((128, 128), ml_dtypes.bfloat16)
    c["ident"] = np.eye(128, dtype=np.float32).astype(ml_dtypes.bfloat16)
    sel = np.zeros((128, 64), np.float32)
    sel[64, :] = 1.0
    c["sel"] = sel
    oh = np.zeros((32, 128, 2, 128), np.float32)
    kk = np.arange(128)[:, None]
    qq = np.arange(128)[None, :]
    for dl in range(2):
        dist = np.maximum(128 * dl + qq - kk, 0)
        bk = t5_bucket_np(dist)
        for b in range(32):
            oh[b, :, dl, :] = (bk == b)
    c["oh"] = oh
    qi = np.arange(128)[:, None]
    ki = np.arange(128)[None, :]
    c["causal_add"] = np.where(ki <= qi, 0.0, NEG).astype(np.float32)
    c["pow2"] = np.tile((0.5 ** np.arange(1, NIT + 1)).astype(np.float32)[None, :], (128, 1))
    return c


CONST_SPECS = [("cosT", None, F32), ("sinT", None, F32), ("mask_incl", (4, 128, 512), BF16),
               ("mask_strict", (4, 128, 512), BF16), ("tri", (128, 128), BF16), ("ones", (128, 128), BF16),
               ("ident", (128, 128), BF16), ("sel", (128, 64), F32), ("oh", (32, 128, 2, 128), F32),
               ("causal_add", (128, 128), F32), ("pow2", (128, NIT), F32)]


def prep_weights(inp):
    w = {}
    w_in = inp["w_in"]
    L = w_in.shape[0]
    w["w_in"] = np.ascontiguousarray(w_in)
    w["w_tok"] = np.ascontiguousarray(np.concatenate(
        [w_in[:, :, O_DV:O_DV + 64], w_in[:, :, O_SV:O_SV + 320], w_in[:, :, O_IW:O_IW + 8]], axis=2))
    wkr = np.zeros((L, D, 2, 96), np.float32)
    wkr[:, :, 0, 64:96] = w_in[:, :, O_KR:O_KR + 32]
    wkr[:, :, 1, 64:80] = w_in[:, :, O_KR + 16:O_KR + 32]
    wkr[:, :, 1, 80:96] = w_in[:, :, O_KR:O_KR + 16]
    w["w_kr"] = wkr
    wuq = inp["mla_w_uq"]
    w["w_uq"] = np.ascontiguousarray(wuq)
    sw = wuq.reshape(L, 384, 6, 96).copy()
    sw[:, :, :, 64:80] = wuq.reshape(L, 384, 6, 96)[:, :, :, 80:96]
    sw[:, :, :, 80:96] = wuq.reshape(L, 384, 6, 96)[:, :, :, 64:80]
    w["w_uqs"] = np.ascontiguousarray(sw.reshape(L, 384, 576))
    wukv = inp["mla_w_ukv"].reshape(L, 256, 6, 128)
    w["w_uk"] = np.ascontiguousarray(wukv[:, :, :, :64].reshape(L, 256, 384))
    w["w_uv"] = np.ascontiguousarray(wukv[:, :, :, 64:].reshape(L, 256, 384))
    g = lambda a, n: np.ascontiguousarray(a.reshape(L, n, 128).transpose(0, 2, 1))
    w["g_attn"] = g(inp["attn_norm"], 8)
    w["g_ffn"] = g(inp["ffn_norm"], 8)
    w["g_ple"] = g(inp["ple_norm"], 8)
    w["g_q"] = g(inp["mla_q_norm"], 3)
    w["g_kv"] = g(inp["mla_kv_norm"], 2)
    w["g_fin"] = np.ascontiguousarray(inp["final_norm"].reshape(8, 128).T)
    for k in ("w_o", "w_gate", "w_up", "w_down", "w_ple_gate", "w_ple_proj", "rel_bias"):
        w[k] = np.ascontiguousarray(inp[k])
    return w


def build(S, depth, debug=False, upto=99):
    NG = S // 512
    NBLK = S // 128
    nc = bass.Bass("TRN2", target_bir_lowering=False)
    dt_in = lambda name, shape, dt=F32: nc.dram_tensor(name, list(shape), dt, kind="ExternalInput").ap()
    dkind = "ExternalOutput" if debug else "Internal"
    dt_sc = lambda name, shape, dt: nc.dram_tensor(name, list(shape), dt, kind=dkind).ap()

    xT = dt_in("xT", (D, S))
    pT = dt_in("pT", (depth, 256, S))
    W = {}
    for name, shape in [("w_in", (depth, D, D_IN)), ("w_tok", (depth, D, 392)), ("w_kr", (depth, D, 2, 96)),
                        ("w_uq", (depth, 384, 576)), ("w_uqs", (depth, 384, 576)), ("w_uk", (depth, 256, 384)),
                        ("w_uv", (depth, 256, 384)), ("g_attn", (depth, 128, 8)), ("g_ffn", (depth, 128, 8)),
                        ("g_ple", (depth, 128, 8)), ("g_q", (depth, 128, 3)), ("g_kv", (depth, 128, 2)),
                        ("g_fin", (128, 8)), ("w_o", (depth, D, D)), ("w_gate", (depth, D, D_FF)),
                        ("w_up", (depth, D, D_FF)), ("w_down", (depth, D_FF, D)), ("w_ple_gate", (depth, D, D)),
                        ("w_ple_proj", (depth, 256, D)), ("rel_bias", (32, 5))]:
        W[name] = dt_in(name, shape)
    C = {}
    for name, shape, dt in CONST_SPECS:
        if shape is None:
            shape = (32, S)
        C[name] = dt_in("c_" + name, shape, dt)
    yT = nc.dram_tensor("yT", [D, S], F32, kind="ExternalOutput").ap()

    hT = dt_sc("hT", (D, S), F32)
    projT = dt_sc("projT", (D_IN, S), BF16)
    mlaQT = dt_sc("mlaQT", (6, 96, S), BF16)
    mlaKT = dt_sc("mlaKT", (6, 96, S), BF16)
    mlaV = dt_sc("mlaV", (6, 128, NBLK, 66), BF16)
    dsaV = dt_sc("dsaV", (128, NBLK, 66), BF16)
    sbV = dt_sc("sbV", (5, 128, NBLK, 64), BF16)
    idxW = dt_sc("idxW", (NBLK, 128, 8), F32)
    mixT = dt_sc("mixT", (D, S), BF16)
    actT = dt_sc("actT", (D_FF, S), BF16)

    from contextlib import ExitStack
    top = ExitStack()
    with top:
        NSP, NPQ = 16, 8
        csems = {e: top.enter_context(nc.semaphore("s_" + e)) for e in ("pe", "act", "dve", "pool")}
        dsems = {"sp": [top.enter_context(nc.semaphore("d_sp%d" % i)) for i in range(NSP)]}
        em = Em(nc, csems, dsems)

        uid = [0]

        def sb(stack, name, shape, dt):
            uid[0] += 1
            return stack.enter_context(nc.sbuf_tensor("%s_%d" % (name, uid[0]), list(shape), dt))

        def ps(stack, name, shape=(128, 512), dt=F32):
            uid[0] += 1
            return stack.enter_context(nc.psum_tensor("%s_%d" % (name, uid[0]), list(shape), dt))

        ones = T(sb(top, "k_ones", (128, 128), BF16))
        tri = T(sb(top, "k_tri", (128, 128), BF16))
        ident = T(sb(top, "k_ident", (128, 128), BF16))
        sel = T(sb(top, "k_sel", (128, 64), F32))
        mincl = T(sb(top, "k_mincl", (128, 4, 512), BF16))
        mstr = T(sb(top, "k_mstr", (128, 4, 512), BF16))
        cadd = T(sb(top, "k_cadd", (128, 128), F32))
        pow2 = T(sb(top, "k_pow2", (128, NIT), F32))
        ctab = T(sb(top, "k_ctab", (128, 2, 5, 128), BF16))
        b31 = T(sb(top, "k_b31", (128, 5), F32))

        with ExitStack() as st:
            oh = T(sb(st, "s_oh", (128, 32, 256), F32))
            rbb = T(sb(st, "s_rbb", (128, 160), F32))
            dcol = T(sb(st, "s_dcol", (128, 32, 5), F32))
            acc = T(sb(st, "s_acc", (128, 5, 256), F32))
            em.dma("sp", ones.t[:], C["ones"], writes=[ones])
            em.dma("sp", tri.t[:], C["tri"], writes=[tri])
            em.dma("sp", ident.t[:], C["ident"], writes=[ident])
            em.dma("sp", sel.t[:], C["sel"], writes=[sel])
            em.dma("sp", mincl.t[:], C["mask_incl"].rearrange("a p q -> p a q"), writes=[mincl])
            em.dma("sp", mstr.t[:], C["mask_strict"].rearrange("a p q -> p a q"), writes=[mstr])
            em.dma("sp", cadd.t[:], C["causal_add"], writes=[cadd])
            em.dma("sp", pow2.t[:], C["pow2"], writes=[pow2])
            for b in range(32):
                em.dma("sp", oh.t[:, b, :], C["oh"][b].rearrange("p a q -> p (a q)"), writes=[oh])
            rb_flat = W["rel_bias"].rearrange("b h -> (b h)")
            em.dma("sp", rbb.t[:], bass.AP(rb_flat.tensor, rb_flat.offset, [[0, 128], [1, 160]]), writes=[rbb])
            rb3 = rbb.t[:].rearrange("p (b h) -> p b h", h=5)
            em.op("dve", lambda e: e.tensor_copy(b31.t[:], rb3[:, 31, :]), reads=[rbb], writes=[b31])
            for h in range(5):
                em.op("dve", lambda e, h=h: e.tensor_scalar(dcol.t[:, :, h], rb3[:, :, h], b31.t[:, h:h + 1], None,
                                                            ALU.subtract), reads=[rbb, b31], writes=[dcol])
            for h in range(5):
                for b in range(31):
                    if b == 0:
                        em.op("dve", lambda e, h=h, b=b: e.tensor_scalar(
                            acc.t[:, h, :], oh.t[:, b, :], dcol.t[:, b, h:h + 1], None, ALU.mult),
                            reads=[oh, dcol], writes=[acc])
                    else:
                        em.op("dve", lambda e, h=h, b=b: e.scalar_tensor_tensor(
                            acc.t[:, h, :], oh.t[:, b, :], dcol.t[:, b, h:h + 1], acc.t[:, h, :], ALU.mult, ALU.add),
                            reads=[oh, dcol, acc], writes=[acc])
            for h in range(5):
                for dl in range(2):
                    em.op("act", lambda e, h=h, dl=dl: e.activation(
                        out=ctab.t[:, dl, h, :], in_=acc.t[:, h, dl * 128:(dl + 1) * 128], func=AF.Exp),
                        reads=[acc], writes=[ctab])
            em.flush()
        if upto < 1:
            return nc, em.nins

        WST = 1408

        def wload(dst, src, nchunk, wst):
            v = src.rearrange("(c p) n -> p c n", p=128)
            n = v.shape[2]
            for c_ in range(nchunk):
                for n0 in range(0, n, WST):
                    n1 = min(n, n0 + WST)
                    s_ = wst.next()
                    em.dma("sp", s_.t[:, 0:n1 - n0], v[:, c_, n0:n1], writes=[s_])
                    em.op("pool", lambda e, s_=s_, c_=c_, n0=n0, n1=n1: e.tensor_copy(dst.t[:, c_, n0:n1], s_.t[:, 0:n1 - n0]),
                          reads=[s_], writes=[dst])

        def fm_norm(hin, gain, nchunk, sqt, psb, rs, scale, eps, hn=None, out_f32=None):
            em.op("act", lambda e: e.activation(out=sqt.t[:, 0:nchunk, :], in_=hin.t[:, 0:nchunk, :], func=AF.Square),
                  reads=[hin], writes=[sqt])
            for c_ in range(nchunk):
                em.op("pe", lambda e, c_=c_: e.matmul(psb.t[:], ones.t[:], sqt.t[:, c_, :],
                                                      start=(c_ == 0), stop=(c_ == nchunk - 1)),
                      reads=[ones, sqt], writes=[psb])
            em.op("dve", lambda e: e.tensor_scalar(rs.t[:], psb.t[:], scale, eps, ALU.mult, ALU.add),
                  reads=[psb], writes=[rs])
            em.op("act", lambda e: e.activation(out=rs.t[:], in_=rs.t[:], func=AF.Sqrt), reads=[rs], writes=[rs])
            em.op("dve", lambda e: e.reciprocal(rs.t[:], rs.t[:]), reads=[rs], writes=[rs])
            if hn is not None:
                for c_ in range(nchunk):
                    em.op("dve", lambda e, c_=c_: e.scalar_tensor_tensor(
                        hn.t[:, c_, :], hin.t[:, c_, :], gain.t[:, c_:c_ + 1], rs.t[:], ALU.mult, ALU.mult),
                        reads=[hin, gain, rs], writes=[hn])

        hview = lambda ap, t0, n=512: ap.rearrange("(c p) t -> p c t", p=128)[:, :, t0:t0 + n]

        for L in range(depth):
            hsrc = xT if L == 0 else hT
            with ExitStack() as st:
                win = T(sb(st, "a_win", (128, 8, D_IN), BF16))
                wtok = T(sb(st, "a_wtok", (128, 8, 392), BF16))
                wkr = T(sb(st, "a_wkr", (128, 8, 192), BF16))
                wuq = T(sb(st, "a_wuq", (128, 3, 576), BF16))
                wuqs = T(sb(st, "a_wuqs", (128, 3, 576), BF16))
                wuk = T(sb(st, "a_wuk", (128, 2, 384), BF16))
                wuv = T(sb(st, "a_wuv", (128, 2, 384), BF16))
                gat = T(sb(st, "a_gat", (128, 8), F32))
                gq = T(sb(st, "a_gq", (128, 3), F32))
                gkv = T(sb(st, "a_gkv", (128, 2), F32))
                hin = T(sb(st, "a_hin", (128, 8, 512), F32))
                sqt = T(sb(st, "a_sq", (128, 8, 512), BF16))
                hn = T(sb(st, "a_hn", (128, 8, 512), BF16))
                rs = T(sb(st, "a_rs", (128, 512), F32))
                rq = T(sb(st, "a_rq", (128, 512), F32))
                rkv = T(sb(st, "a_rkv", (128, 512), F32))
                cs = T(sb(st, "a_cs", (128, 2, 512), F32))
                raw = T(sb(st, "a_raw", (128, 5, 512), F32))
                sq2 = T(sb(st, "a_sq2", (128, 5, 512), BF16))
                cqn = T(sb(st, "a_cqn", (128, 3, 512), BF16))
                ckvn = T(sb(st, "a_ckvn", (128, 2, 512), BF16))
                stg = Ring([sb(st, "a_stg%d" % i, (128, 512), BF16) for i in range(4)])
                t1r = Ring([sb(st, "a_t1%d" % i, (128, 512), F32) for i in range(2)])
                t2r = Ring([sb(st, "a_t2%d" % i, (128, 512), F32) for i in range(2)])
                dvt = T(sb(st, "a_dvt", (128, 4, 66), BF16))
                svt = T(sb(st, "a_svt", (128, 4, 320), BF16))
                iwt = T(sb(st, "a_iwt", (128, 4, 8), F32))
                mvt = T(sb(st, "a_mvt", (128, 4, 6, 66), BF16))
                psr = Ring([ps(st, "a_ps%d" % i) for i in range(6)])
                pss = T(ps(st, "a_pss"))
                pss2 = T(ps(st, "a_pss2"))

                ABITS0 = int(os.environ.get('A_BITS', '63'))
                if not (ABITS0 & 64):
                    wst = Ring([sb(st, "a_wst%d" % i, (128, WST), F32) for i in range(2)])
                    wload(win, W["w_in"][L], 8, wst)
                    wload(wtok, W["w_tok"][L], 8, wst)
                    wload(wkr, W["w_kr"][L].rearrange("d a m -> d (a m)"), 8, wst)
                    wload(wuq, W["w_uq"][L], 3, wst)
                    wload(wuqs, W["w_uqs"][L], 3, wst)
                    wload(wuk, W["w_uk"][L], 2, wst)
                    wload(wuv, W["w_uv"][L], 2, wst)
                em.dma("sp", gat.t[:], W["g_attn"][L], writes=[gat])
                em.dma("sp", gq.t[:], W["g_q"][L], writes=[gq])
                em.dma("sp", gkv.t[:], W["g_kv"][L], writes=[gkv])
                em.op("pool", lambda e: e.memset(dvt.t[:], 1.0), writes=[dvt])
                em.op("pool", lambda e: e.memset(mvt.t[:], 1.0), writes=[mvt])
                if os.environ.get("SPLITW", "1") == "1":
                    em.flush()
                evq = [0]

                def evac(dst_ap, src_ap, reads, writes, scale=None):
                    evq[0] += 1
                    if evq[0] % 2 == 0:
                        if scale is None:
                            em.op("act", lambda e: e.copy(dst_ap, src_ap), reads=reads, writes=writes)
                        else:
                            em.op("act", lambda e: e.mul(dst_ap, src_ap, scale), reads=reads, writes=writes)
                    else:
                        if scale is None:
                            em.op("dve", lambda e: e.tensor_copy(dst_ap, src_ap), reads=reads, writes=writes)
                        else:
                            em.op("dve", lambda e: e.tensor_scalar(dst_ap, src_ap, scale, None, ALU.mult),
                                  reads=reads, writes=writes)

                apasses = [int(x) for x in os.environ.get('A_PASSES', '63').split(',')]
                for (ABITS, g) in [(ab, g_) for ab in apasses for g_ in range(NG)]:
                    if g == 0 and ABITS != apasses[0]:
                        em.flush()
                    t0 = g * 512
                    em.dma("sp", hin.t[:], hview(hsrc, t0), writes=[hin])
                    em.dma("sp", cs.t[64:96, 0, :], C["cosT"][:, t0:t0 + 512], writes=[cs])
                    em.dma("sp", cs.t[64:96, 1, :], C["sinT"][:, t0:t0 + 512], writes=[cs])
                    fm_norm(hin, gat, 8, sqt, pss, rs, 1.0 / D, EPS, hn=hn)
                    for (c0, n, scale) in [] if not (ABITS & 1) else [(O_DQ, 320, 0.125), (O_DK, 64, None), (O_IQ, 512, None), (O_IK, 64, None),
                                           (O_SQ, 320, 0.125), (O_SK, 320, None)]:
                        m0 = 0
                        while m0 < n:
                            M = min(128, n - m0)
                            p = psr.next()
                            for c_ in range(8):
                                em.op("pe", lambda e, p=p, c_=c_, a=c0 + m0, M=M: e.matmul(
                                    p.t[0:M, :], win.t[:, c_, a:a + M], hn.t[:, c_, :], start=(c_ == 0), stop=(c_ == 7)),
                                    reads=[win, hn], writes=[p])
                            o = stg.next()
                            evac(o.t[0:M, :], p.t[0:M, :], [p], [o], scale)
                            em.dma("sp", projT[c0 + m0:c0 + m0 + M, t0:t0 + 512], o.t[0:M, :], reads=[o])
                            m0 += M
                    if not (ABITS & 2):
                        continue
                    for j in range(5):
                        p = psr.next()
                        for c_ in range(8):
                            em.op("pe", lambda e, p=p, c_=c_, j=j: e.matmul(
                                p.t[:], win.t[:, c_, j * 128:(j + 1) * 128], hn.t[:, c_, :], start=(c_ == 0), stop=(c_ == 7)),
                                reads=[win, hn], writes=[p])
                        evac(raw.t[:, j, :], p.t[:], [p], [raw])
                    em.op("act", lambda e: e.activation(out=sq2.t[:], in_=raw.t[:], func=AF.Square), reads=[raw], writes=[sq2])
                    for j in range(3):
                        em.op("pe", lambda e, j=j: e.matmul(pss.t[:], ones.t[:], sq2.t[:, j, :], start=(j == 0), stop=(j == 2)),
                              reads=[ones, sq2], writes=[pss])
                    for j in range(2):
                        em.op("pe", lambda e, j=j: e.matmul(pss2.t[:], ones.t[:], sq2.t[:, 3 + j, :], start=(j == 0), stop=(j == 1)),
                              reads=[ones, sq2], writes=[pss2])
                    for (r_, p_, sc_, ep_) in [(rq, pss, 96.0 / 384.0, 96.0 * EPS), (rkv, pss2, 1.0 / 256.0, EPS)]:
                        em.op("dve", lambda e, r_=r_, p_=p_, sc_=sc_, ep_=ep_: e.tensor_scalar(
                            r_.t[:], p_.t[:], sc_, ep_, ALU.mult, ALU.add), reads=[p_], writes=[r_])
                        em.op("act", lambda e, r_=r_: e.activation(out=r_.t[:], in_=r_.t[:], func=AF.Sqrt), reads=[r_], writes=[r_])
                        em.op("dve", lambda e, r_=r_: e.reciprocal(r_.t[:], r_.t[:]), reads=[r_], writes=[r_])
                    for j in range(3):
                        em.op("dve", lambda e, j=j: e.scalar_tensor_tensor(
                            cqn.t[:, j, :], raw.t[:, j, :], gq.t[:, j:j + 1], rq.t[:], ALU.mult, ALU.mult),
                            reads=[raw, gq, rq], writes=[cqn])
                    for j in range(2):
                        em.op("dve", lambda e, j=j: e.scalar_tensor_tensor(
                            ckvn.t[:, j, :], raw.t[:, 3 + j, :], gkv.t[:, j:j + 1], rkv.t[:], ALU.mult, ALU.mult),
                            reads=[raw, gkv, rkv], writes=[ckvn])

                    def rope_out(pa, pb, o):
                        a_, b_ = t1r.next(), t2r.next()
                        em.op("dve", lambda e: e.tensor_tensor(a_.t[64:96, :], pa.t[64:96, :], cs.t[64:96, 0, :], ALU.mult),
                              reads=[pa, cs], writes=[a_])
                        em.op("dve", lambda e: e.tensor_tensor(b_.t[64:96, :], pb.t[64:96, :], cs.t[64:96, 1, :], ALU.mult),
                              reads=[pb, cs], writes=[b_])
                        em.op("pool", lambda e: e.tensor_tensor(o.t[64:96, :], a_.t[64:96, :], b_.t[64:96, :], ALU.add),
                              reads=[a_, b_], writes=[o])

                    for h in range(6 if (ABITS & 4) else 0):
                        pa, pb = psr.next(), psr.next()
                        for (p, w_) in [(pa, wuq), (pb, wuqs)]:
                            for j in range(3):
                                em.op("pe", lambda e, p=p, w_=w_, j=j, h=h: e.matmul(
                                    p.t[0:96, :], w_.t[:, j, h * 96:(h + 1) * 96], cqn.t[:, j, :], start=(j == 0), stop=(j == 2)),
                                    reads=[w_, cqn], writes=[p])
                        o = stg.next()
                        em.op("act", lambda e, o=o, pa=pa: e.copy(o.t[0:64, :], pa.t[0:64, :]), reads=[pa], writes=[o])
                        rope_out(pa, pb, o)
                        em.dma("sp", mlaQT[h, :, t0:t0 + 512], o.t[0:96, :], reads=[o])
                    for h in range(6 if (ABITS & 8) else 0):
                        p = psr.next()
                        for j in range(2):
                            em.op("pe", lambda e, p=p, j=j, h=h: e.matmul(
                                p.t[0:64, :], wuk.t[:, j, h * 64:(h + 1) * 64], ckvn.t[:, j, :], start=(j == 0), stop=(j == 1)),
                                reads=[wuk, ckvn], writes=[p])
                        o = stg.next()
                        evac(o.t[0:64, :], p.t[0:64, :], [p], [o])
                        em.dma("sp", mlaKT[h, 0:64, t0:t0 + 512], o.t[0:64, :], reads=[o])
                    if ABITS & 16:
                        pa, pb = psr.next(), psr.next()
                        for (p, v_) in [(pa, 0), (pb, 1)]:
                            for c_ in range(8):
                                em.op("pe", lambda e, p=p, v_=v_, c_=c_: e.matmul(
                                    p.t[0:96, :], wkr.t[:, c_, v_ * 96:(v_ + 1) * 96], hn.t[:, c_, :], start=(c_ == 0), stop=(c_ == 7)),
                                    reads=[wkr, hn], writes=[p])
                        o = stg.next()
                        rope_out(pa, pb, o)
                        for h in range(6):
                            em.dma("sp", mlaKT[h, 64:96, t0:t0 + 512], o.t[64:96, :], reads=[o])
                    if not (ABITS & 32):
                        continue
                    for blk in range(4):
                        if not (ABITS & 4096):
                            p = psr.next()
                            for c_ in range(8):
                                em.op("pe", lambda e, p=p, c_=c_, blk=blk: e.matmul(
                                    p.t[:, 0:392], hn.t[:, c_, blk * 128:(blk + 1) * 128], wtok.t[:, c_, :], start=(c_ == 0), stop=(c_ == 7)),
                                    reads=[wtok, hn], writes=[p])
                            if not (ABITS & 16384):
                                em.op("dve", lambda e, p=p, blk=blk: e.tensor_copy(dvt.t[:, blk, 0:64], p.t[:, 0:64]), reads=[p], writes=[dvt])
                            em.op("dve", lambda e, p=p, blk=blk: e.tensor_copy(svt.t[:, blk, :], p.t[:, 64:384]), reads=[p], writes=[svt])
                            if not (ABITS & 32768):
                                em.op("dve", lambda e, p=p, blk=blk: e.tensor_copy(iwt.t[:, blk, :], p.t[:, 384:392]), reads=[p], writes=[iwt])
                        if not (ABITS & 8192):
                            p2 = psr.next()
                            for j in range(2):
                                em.op("pe", lambda e, p2=p2, j=j, blk=blk: e.matmul(
                                    p2.t[:, 0:384], ckvn.t[:, j, blk * 128:(blk + 1) * 128], wuv.t[:, j, :], start=(j == 0), stop=(j == 1)),
                                    reads=[wuv, ckvn], writes=[p2])
                            em.op("dve", lambda e, p2=p2, blk=blk: e.tensor_copy(
                                mvt.t[:, blk, :, 0:64], p2.t[:, 0:384].rearrange("p (h d) -> p h d", d=64)), reads=[p2], writes=[mvt])
                    if not (ABITS & 256):
                        em.dma("sp", dsaV[:, 4 * g:4 * g + 4, :], dvt.t[:], reads=[dvt])
                    if not (ABITS & 512):
                        for h in range(5):
                            em.dma("sp", sbV[h, :, 4 * g:4 * g + 4, :], svt.t[:, :, h * 64:(h + 1) * 64], reads=[svt])
                    if not (ABITS & 1024):
                        for h in range(6):
                            em.dma("sp", mlaV[h, :, 4 * g:4 * g + 4, :], mvt.t[:, :, h, :], reads=[mvt])
                    if not (ABITS & 2048):
                        em.dma("sp", idxW.rearrange("b p h -> p b h")[:, 4 * g:4 * g + 4, :], iwt.t[:], reads=[iwt])
                em.flush()

            if upto < 2:
                return nc, em.nins
            with ExitStack() as st:
                ktr = Ring([sb(st, "m_kt%d" % i, (96, S), BF16) for i in range(2)])
                qtr = Ring([sb(st, "m_qt%d" % i, (96, S), BF16) for i in range(2)])
                vr = Ring([sb(st, "m_v%d" % i, (128, NBLK, 66), BF16) for i in range(2)])
                ptr_ = Ring([sb(st, "m_pt%d" % i, (128, 512), BF16) for i in range(3)])
                osb = T(sb(st, "m_osb", (128, 512), F32))
                rb_ = T(sb(st, "m_rb", (64, 512), F32))
                mo = Ring([sb(st, "m_mo%d" % i, (64, 512), BF16) for i in range(2)])
                pss_ = Ring([ps(st, "m_ps%d" % i) for i in range(3)])
                pso = T(ps(st, "m_pso"))
                psb_ = T(ps(st, "m_psb"))
                for h in range(6):
                    kt, qt, v = ktr.next(), qtr.next(), vr.next()
                    em.dma("sp", kt.t[:], mlaKT[h], writes=[kt])
                    em.dma("sp", qt.t[:], mlaQT[h], writes=[qt])
                    em.dma("sp", v.t[:], mlaV[h], writes=[v])
                    for G in range(NG):
                        t0 = G * 512
                        nkb = 4 * (G + 1)
                        for kb in range(nkb):
                            p = pss_.next()
                            em.op("pe", lambda e, p=p, kt=kt, qt=qt, kb=kb, t0=t0: e.matmul(
                                p.t[:], kt.t[0:96, kb * 128:(kb + 1) * 128], qt.t[0:96, t0:t0 + 512], start=True, stop=True),
                                reads=[kt, qt], writes=[p])
                            pt = ptr_.next()
                            em.op("act", lambda e, p=p, pt=pt: e.activation(out=pt.t[:], in_=p.t[:], func=AF.Exp),
                                  reads=[p], writes=[pt])
                            if kb >= 4 * G:
                                em.op("pool", lambda e, pt=pt, kb=kb, G=G: e.tensor_tensor(
                                    pt.t[:], pt.t[:], mincl.t[:, kb - 4 * G, :], ALU.mult), reads=[pt, mincl], writes=[pt])
                            em.op("pe", lambda e, pt=pt, v=v, kb=kb, nkb=nkb: e.matmul(
                                pso.t[0:65, :], v.t[:, kb, 0:65], pt.t[:], start=(kb == 0), stop=(kb == nkb - 1)),
                                reads=[v, pt], writes=[pso])
                        em.op("act", lambda e: e.copy(osb.t[0:65, :], pso.t[0:65, :]), reads=[pso], writes=[osb])
                        em.op("pe", lambda e: e.matmul(psb_.t[0:64, :], sel.t[0:65, :], osb.t[0:65, :], start=True, stop=True),
                              reads=[sel, osb], writes=[psb_])
                        em.op("dve", lambda e: e.reciprocal(rb_.t[:], psb_.t[0:64, :]), reads=[psb_], writes=[rb_])
                        m = mo.next()
                        em.op("dve", lambda e, m=m: e.tensor_tensor(m.t[:], osb.t[0:64, :], rb_.t[:], ALU.mult),
                              reads=[osb, rb_], writes=[m])
                        em.dma("sp", mixT[M_MLA + 64 * h:M_MLA + 64 * h + 64, t0:t0 + 512], m.t[:], reads=[m])
                em.flush()

            if upto < 3:
                return nc, em.nins
            with ExitStack() as st:
                ktr = Ring([sb(st, "s_kt%d" % i, (64, S), BF16) for i in range(2)])
                qtr = Ring([sb(st, "s_qt%d" % i, (64, S), BF16) for i in range(2)])
                nqr = Ring([sb(st, "s_nq%d" % i, (64, S), BF16) for i in range(2)])
                vr = Ring([sb(st, "s_v%d" % i, (128, NBLK, 64), BF16) for i in range(2)])
                er = Ring([sb(st, "s_e%d" % i, (128, 512), F32) for i in range(2)])
                spr = Ring([sb(st, "s_sp%d" % i, (128, 512), BF16) for i in range(3)])
                wr = Ring([sb(st, "s_w%d" % i, (128, 512), BF16) for i in range(3)])
                R = T(sb(st, "s_R", (128, 512), BF16))
                mo = Ring([sb(st, "s_mo%d" % i, (64, 512), BF16) for i in range(2)])
                psz = Ring([ps(st, "s_psz%d" % i) for i in range(2)])
                psl = Ring([ps(st, "s_psl%d" % i) for i in range(2)])
                pso = T(ps(st, "s_pso"))
                for h in range(5):
                    kt, qt, nq, v = ktr.next(), qtr.next(), nqr.next(), vr.next()
                    em.dma("sp", kt.t[:], projT[O_SK + 64 * h:O_SK + 64 * h + 64, :], writes=[kt])
                    em.dma("sp", qt.t[:], projT[O_SQ + 64 * h:O_SQ + 64 * h + 64, :], writes=[qt])
                    em.dma("sp", v.t[:], sbV[h], writes=[v])
                    em.op("dve", lambda e, nq=nq, qt=qt: e.tensor_scalar(nq.t[:], qt.t[:], -1.0, None, ALU.mult),
                          reads=[qt], writes=[nq])
                    for G in range(NG):
                        t0 = G * 512
                        nkb = 4 * (G + 1)
                        for kb in range(nkb - 1, -1, -1):
                            first = (kb == nkb - 1)
                            diag = kb >= 4 * G
                            pz = psz.next()
                            em.op("pe", lambda e, pz=pz, kt=kt, qt=qt, kb=kb, t0=t0: e.matmul(
                                pz.t[:], kt.t[:, kb * 128:(kb + 1) * 128], qt.t[:, t0:t0 + 512], start=True, stop=True),
                                reads=[kt, qt], writes=[pz])
                            E = er.next()
                            em.op("act", lambda e, pz=pz, E=E: e.activation(out=E.t[:], in_=pz.t[:], func=AF.Exp),
                                  reads=[pz], writes=[E])
                            SP = spr.next()
                            em.op("act", lambda e, SP=SP, E=E: e.activation(out=SP.t[:], in_=E.t[:], func=AF.Ln, bias=1.0),
                                  reads=[E], writes=[SP])
                            if diag:
                                em.op("pool", lambda e, SP=SP, kb=kb, G=G: e.tensor_tensor(
                                    SP.t[:], SP.t[:], mstr.t[:, kb - 4 * G, :], ALU.mult), reads=[SP, mstr], writes=[SP])
                            pl = psl.next()
                            em.op("pe", lambda e, pl=pl, SP=SP: e.matmul(pl.t[:], tri.t[:], SP.t[:], start=True, stop=False),
                                  reads=[tri, SP], writes=[pl])
                            if not first:
                                em.op("pe", lambda e, pl=pl: e.matmul(pl.t[:], ones.t[:], R.t[:], start=False, stop=False),
                                      reads=[ones, R], writes=[pl])
                            em.op("pe", lambda e, pl=pl, kt=kt, nq=nq, kb=kb, t0=t0: e.matmul(
                                pl.t[:], kt.t[:, kb * 128:(kb + 1) * 128], nq.t[:, t0:t0 + 512], start=False, stop=True),
                                reads=[kt, nq], writes=[pl])
                            Wt = wr.next()
                            em.op("act", lambda e, pl=pl, Wt=Wt: e.activation(out=Wt.t[:], in_=pl.t[:], func=AF.Exp, scale=-1.0),
                                  reads=[pl], writes=[Wt])
                            if diag:
                                em.op("pool", lambda e, Wt=Wt, kb=kb, G=G: e.tensor_tensor(
                                    Wt.t[:], Wt.t[:], mstr.t[:, kb - 4 * G, :], ALU.mult), reads=[Wt, mstr], writes=[Wt])
                            em.op("pe", lambda e, Wt=Wt, v=v, kb=kb, first=first: e.matmul(
                                pso.t[0:64, :], v.t[:, kb, :], Wt.t[:], start=first, stop=(kb == 0)),
                                reads=[v, Wt], writes=[pso])
                            if kb > 0:
                                if first:
                                    em.op("pool", lambda e, SP=SP: e.tensor_copy(R.t[:], SP.t[:]), reads=[SP], writes=[R])
                                else:
                                    em.op("pool", lambda e, SP=SP: e.tensor_tensor(R.t[:], R.t[:], SP.t[:], ALU.add),
                                          reads=[SP, R], writes=[R])
                        m = mo.next()
                        em.op("dve", lambda e, m=m: e.tensor_copy(m.t[:], pso.t[0:64, :]), reads=[pso], writes=[m])
                        em.dma("sp", mixT[M_SB + 64 * h:M_SB + 64 * h + 64, t0:t0 + 512], m.t[:], reads=[m])
                em.flush()

            if upto < 4:
                return nc, em.nins
            with ExitStack() as st:
                ikT = T(sb(st, "d_ikT", (64, S), BF16))
                dkT = T(sb(st, "d_dkT", (64, S), BF16))
                dv = T(sb(st, "d_dv", (128, NBLK, 66), BF16))
                isc = T(sb(st, "d_isc", (128, S), F32))
                msk = T(sb(st, "d_msk", (128, S), BF16))
                mT = T(sb(st, "d_mT", (128, NBLK, 256), BF16))
                iqr = Ring([sb(st, "d_iq%d" % i, (64, 8, 128), BF16) for i in range(2)])
                iwr = Ring([sb(st, "d_iw%d" % i, (128, 8), F32) for i in range(2)])
                dqr = Ring([sb(st, "d_dq%d" % i, (64, 5, 256), BF16) for i in range(2)])
                rr = Ring([sb(st, "d_r%d" % i, (128, 512), BF16) for i in range(3)])
                ptr_ = Ring([sb(st, "d_pt%d" % i, (128, 256), BF16) for i in range(3)])
                sm = T(sb(st, "d_sm", (128, 16), F32))
                wi = T(sb(st, "d_wi", (128, NIT), F32))
                osb = T(sb(st, "d_osb", (128, 256), F32))
                rb_ = T(sb(st, "d_rb", (64, 256), F32))
                mo = Ring([sb(st, "d_mo%d" % i, (64, 256), BF16) for i in range(2)])
                psi = Ring([ps(st, "d_psi%d" % i) for i in range(2)])
                psa = T(ps(st, "d_psa"))
                dgr = Ring([sb(st, "d_dg%d" % i, (128, 8, 128), BF16) for i in range(2)])
                pst = T(ps(st, "d_pst", (128, 512), BF16))
                pss_ = Ring([ps(st, "d_pss%d" % i, (128, 256)) for i in range(2)])
                pso = T(ps(st, "d_pso", (128, 256)))
                psb_ = T(ps(st, "d_psb", (128, 256)))
                em.dma("sp", ikT.t[:], projT[O_IK:O_IK + 64, :], writes=[ikT])
                em.dma("sp", dkT.t[:], projT[O_DK:O_DK + 64, :], writes=[dkT])
                em.dma("sp", dv.t[:], dsaV, writes=[dv])
                LO, MID, CNT, STEP, MX, MN, THR = 0, 1, 2, 3, 4, 5, 6
                for G2 in range(S // 256):
                    q0 = G2 * 256
                    dq = dqr.next()
                    em.dma("sp", dq.t[:], projT[O_DQ:O_DQ + 320, q0:q0 + 256].rearrange("(h p) t -> p h t", p=64), writes=[dq])
                    for qs in range(2):
                        QB = 2 * G2 + qs
                        qb0 = QB * 128
                        Lk = (QB + 1) * 128
                        iq, iw = iqr.next(), iwr.next()
                        em.dma("sp", iq.t[:], projT[O_IQ:O_IQ + 512, qb0:qb0 + 128].rearrange("(h p) t -> p h t", p=64), writes=[iq])
                        em.dma("sp", iw.t[:], idxW[QB], writes=[iw])
                        nch = (Lk + 511) // 512
                        dg = dgr.next()
                        for h in range(8):
                            em.op("pool", lambda e, dg=dg, iw=iw, h=h: e.tensor_scalar(
                                dg.t[:, h, :], ident.t[:], iw.t[:, h:h + 1], None, ALU.mult), reads=[ident, iw], writes=[dg])
                        for c_ in range(nch):
                            k0 = c_ * 512
                            wc = min(512, Lk - k0)

                            def mm_s(h, k0=k0, wc=wc):
                                p = psi.next()
                                em.op("pe", lambda e, p=p, iq=iq, h=h, k0=k0, wc=wc: e.matmul(
                                    p.t[:, 0:wc], iq.t[:, h, :], ikT.t[:, k0:k0 + wc], start=True, stop=True),
                                    reads=[iq, ikT], writes=[p])
                                return p
                            pcur = mm_s(0)
                            for h in range(8):
                                pnext = mm_s(h + 1) if h < 7 else None
                                r = rr.next()
                                em.op("act", lambda e, p=pcur, r=r, wc=wc: e.activation(out=r.t[:, 0:wc], in_=p.t[:, 0:wc], func=AF.Relu),
                                      reads=[pcur], writes=[r])
                                em.op("pe", lambda e, r=r, dg=dg, h=h, wc=wc: e.matmul(
                                    psa.t[:, 0:wc], dg.t[:, h, :], r.t[:, 0:wc], start=(h == 0), stop=(h == 7)),
                                    reads=[dg, r], writes=[psa])
                                pcur = pnext
                            em.op("act", lambda e, k0=k0, wc=wc: e.copy(isc.t[:, k0:k0 + wc], psa.t[:, 0:wc]), reads=[psa], writes=[isc])
                        em.op("dve", lambda e, qb0=qb0: e.tensor_tensor(
                            isc.t[:, qb0:qb0 + 128], isc.t[:, qb0:qb0 + 128], cadd.t[:], ALU.add), reads=[isc, cadd], writes=[isc])
                        if Lk <= TOPK:
                            em.op("dve", lambda e: e.memset(sm.t[:, THR:THR + 1], -1.0e29), writes=[sm])
                        else:
                            em.op("dve", lambda e, Lk=Lk: e.tensor_reduce(sm.t[:, MX:MX + 1], isc.t[:, 0:Lk], AX.X, ALU.max),
                                  reads=[isc], writes=[sm])
                            em.op("dve", lambda e, Lk=Lk: e.tensor_reduce(sm.t[:, LO:LO + 1], isc.t[:, 0:Lk - 128], AX.X, ALU.min),
                                  reads=[isc], writes=[sm])
                            em.op("dve", lambda e: e.tensor_tensor(sm.t[:, MN:MN + 1], sm.t[:, MX:MX + 1], sm.t[:, LO:LO + 1], ALU.subtract),
                                  reads=[sm], writes=[sm])
                            em.op("dve", lambda e: e.tensor_scalar(wi.t[:], pow2.t[:], sm.t[:, MN:MN + 1], None, ALU.mult),
                                  reads=[sm, pow2], writes=[wi])
                            for it in range(NIT):
                                em.op("dve", lambda e, it=it: e.tensor_tensor(
                                    sm.t[:, MID:MID + 1], sm.t[:, LO:LO + 1], wi.t[:, it:it + 1], ALU.add), reads=[sm, wi], writes=[sm])
                                em.op("dve", lambda e, Lk=Lk: e.tensor_scalar(
                                    msk.t[:, 0:Lk], isc.t[:, 0:Lk], sm.t[:, MID:MID + 1], 0.0, ALU.is_ge, ALU.add,
                                    accum_out=sm.t[:, CNT:CNT + 1]), reads=[isc, sm], writes=[msk, sm])
                                em.op("dve", lambda e, it=it: e.tensor_scalar(
                                    sm.t[:, STEP:STEP + 1], sm.t[:, CNT:CNT + 1], TOPK - 0.5, wi.t[:, it:it + 1], ALU.is_ge, ALU.mult),
                                    reads=[sm, wi], writes=[sm])
                                em.op("dve", lambda e: e.tensor_tensor(
                                    sm.t[:, LO:LO + 1], sm.t[:, LO:LO + 1], sm.t[:, STEP:STEP + 1], ALU.add), reads=[sm], writes=[sm])
                            em.op("dve", lambda e: e.tensor_copy(sm.t[:, THR:THR + 1], sm.t[:, LO:LO + 1]), reads=[sm], writes=[sm])
                        em.op("dve", lambda e, Lk=Lk: e.tensor_scalar(
                            msk.t[:, 0:Lk], isc.t[:, 0:Lk], sm.t[:, THR:THR + 1], None, ALU.is_ge), reads=[isc, sm], writes=[msk])
                        nb = QB + 1
                        for k4 in range(0, nb, 4):
                            n4 = min(4, nb - k4)
                            for i in range(n4):
                                em.op("pe", lambda e, k4=k4, i=i: e.transpose(
                                    pst.t[:, i * 128:(i + 1) * 128], msk.t[:, (k4 + i) * 128:(k4 + i + 1) * 128], ident.t[:]),
                                    reads=[msk, ident], writes=[pst])
                            em.op("act", lambda e, k4=k4, n4=n4, qs=qs: e.copy(
                                mT.t[:, k4:k4 + n4, qs * 128:(qs + 1) * 128],
                                pst.t[:, 0:n4 * 128].rearrange("p (a q) -> p a q", q=128)), reads=[pst], writes=[mT])
                        if qs == 0:
                            em.op("pool", lambda e, QB=QB: e.memset(mT.t[:, QB + 1, 0:128], 0.0), writes=[mT])
                    nkb = 2 * G2 + 2
                    for h in range(5):
                        for kb in range(nkb):
                            p = pss_.next()
                            em.op("pe", lambda e, p=p, dq=dq, h=h, kb=kb: e.matmul(
                                p.t[:, :], dkT.t[:, kb * 128:(kb + 1) * 128], dq.t[:, h, :], start=True, stop=True),
                                reads=[dkT, dq], writes=[p])
                            pt = ptr_.next()
                            em.op("act", lambda e, p=p, pt=pt, h=h: e.activation(
                                out=pt.t[:], in_=p.t[:], func=AF.Exp, bias=b31.t[:, h:h + 1]), reads=[p, b31], writes=[pt])
                            em.op("pool", lambda e, pt=pt, kb=kb: e.tensor_tensor(pt.t[:], pt.t[:], mT.t[:, kb, :], ALU.mult),
                                  reads=[pt, mT], writes=[pt])
                            for qs in range(2):
                                dl = 2 * G2 + qs - kb
                                if dl in (0, 1):
                                    em.op("pool", lambda e, pt=pt, qs=qs, dl=dl, h=h: e.tensor_tensor(
                                        pt.t[:, qs * 128:(qs + 1) * 128], pt.t[:, qs * 128:(qs + 1) * 128], ctab.t[:, dl, h, :], ALU.mult),
                                        reads=[pt, ctab], writes=[pt])
                            em.op("pe", lambda e, pt=pt, kb=kb, nkb=nkb: e.matmul(
                                pso.t[0:65, :], dv.t[:, kb, 0:65], pt.t[:], start=(kb == 0), stop=(kb == nkb - 1)),
                                reads=[dv, pt], writes=[pso])
                        em.op("act", lambda e: e.copy(osb.t[0:65, :], pso.t[0:65, :]), reads=[pso], writes=[osb])
                        em.op("pe", lambda e: e.matmul(psb_.t[0:64, :], sel.t[0:65, :], osb.t[0:65, :], start=True, stop=True),
                              reads=[sel, osb], writes=[psb_])
                        em.op("dve", lambda e: e.reciprocal(rb_.t[:], psb_.t[0:64, :]), reads=[psb_], writes=[rb_])
                        m = mo.next()
                        em.op("dve", lambda e, m=m: e.tensor_tensor(m.t[:], osb.t[0:64, :], rb_.t[:], ALU.mult),
                              reads=[osb, rb_], writes=[m])
                        em.dma("sp", mixT[M_DSA + 64 * h:M_DSA + 64 * h + 64, q0:q0 + 256], m.t[:], reads=[m])
                em.flush()

            if upto < 5:
                return nc, em.nins
            with ExitStack() as st:
                wo = T(sb(st, "c_wo", (128, 8, D), BF16))
                mixr = Ring([sb(st, "c_mix%d" % i, (128, 8, 512), BF16) for i in range(2)])
                hinr = Ring([sb(st, "c_hin%d" % i, (128, 8, 512), F32) for i in range(2)])
                houtr = Ring([sb(st, "c_hout%d" % i, (128, 8, 512), F32) for i in range(2)])
                psr = Ring([ps(st, "c_ps%d" % i) for i in range(4)])
                wst = Ring([sb(st, "c_wst%d" % i, (128, WST), F32) for i in range(2)])
                wload(wo, W["w_o"][L], 8, wst)
                em.flush()
                for g in range(NG):
                    t0 = g * 512
                    mix, hin, hout = mixr.next(), hinr.next(), houtr.next()
                    em.dma("sp", mix.t[:], hview(mixT, t0), writes=[mix])
                    em.dma("sp", hin.t[:], hview(hsrc, t0), writes=[hin])
                    for fc in range(8):
                        p = psr.next()
                        for c_ in range(8):
                            em.op("pe", lambda e, p=p, c_=c_, fc=fc, mix=mix: e.matmul(
                                p.t[:], wo.t[:, c_, fc * 128:(fc + 1) * 128], mix.t[:, c_, :], start=(c_ == 0), stop=(c_ == 7)),
                                reads=[wo, mix], writes=[p])
                        em.op("dve", lambda e, p=p, fc=fc, hin=hin, hout=hout: e.tensor_tensor(
                            hout.t[:, fc, :], p.t[:], hin.t[:, fc, :], ALU.add), reads=[p, hin], writes=[hout])
                    em.dma("sp", hview(hT, t0), hout.t[:], reads=[hout])
                em.flush()

            if upto < 6:
                return nc, em.nins
            with ExitStack() as st:
                wg = T(sb(st, "f_wg", (128, 8, D_FF), BF16))
                wu = T(sb(st, "f_wu", (128, 8, D_FF), BF16))
                gf = T(sb(st, "f_gf", (128, 8), F32))
                hinr = Ring([sb(st, "f_hin%d" % i, (128, 8, 512), F32) for i in range(1)])
                sqt = T(sb(st, "f_sq", (128, 8, 512), BF16))
                hf = T(sb(st, "f_hf", (128, 8, 512), BF16))
                rs = T(sb(st, "f_rs", (128, 512), F32))
                sgr = Ring([sb(st, "f_sg%d" % i, (128, 512), F32) for i in range(2)])
                actr = Ring([sb(st, "f_act%d" % i, (128, NFF, 512), BF16) for i in range(1)])
                pss = T(ps(st, "f_pss"))
                psg = Ring([ps(st, "f_psg%d" % i) for i in range(3)])
                psu = Ring([ps(st, "f_psu%d" % i) for i in range(3)])
                wst = Ring([sb(st, "f_wst%d" % i, (128, WST), F32) for i in range(2)])
                wload(wg, W["w_gate"][L], 8, wst)
                wload(wu, W["w_up"][L], 8, wst)
                em.dma("sp", gf.t[:], W["g_ffn"][L], writes=[gf])
                em.flush()
                for g in range(NG):
                    t0 = g * 512
                    hin, at = hinr.next(), actr.next()
                    em.dma("sp", hin.t[:], hview(hT, t0), writes=[hin])
                    fm_norm(hin, gf, 8, sqt, pss, rs, 1.0 / D, EPS, hn=hf)
                    for f in range(NFF):
                        pg, pu = psg.next(), psu.next()
                        for (p, w_) in [(pg, wg), (pu, wu)]:
                            for c_ in range(8):
                                em.op("pe", lambda e, p=p, w_=w_, c_=c_, f=f: e.matmul(
                                    p.t[:], w_.t[:, c_, f * 128:(f + 1) * 128], hf.t[:, c_, :], start=(c_ == 0), stop=(c_ == 7)),
                                    reads=[w_, hf], writes=[p])
                        sg = sgr.next()
                        em.op("act", lambda e, pg=pg, sg=sg: e.activation(out=sg.t[:], in_=pg.t[:], func=AF.Silu), reads=[pg], writes=[sg])
                        em.op("dve", lambda e, pu=pu, sg=sg, at=at, f=f: e.tensor_tensor(at.t[:, f, :], sg.t[:], pu.t[:], ALU.mult),
                              reads=[pu, sg], writes=[at])
                    em.dma("sp", hview(actT, t0), at.t[:], reads=[at])
                em.flush()

            if upto < 7:
                return nc, em.nins
            with ExitStack() as st:
                last = (L == depth - 1)
                wd = T(sb(st, "g_wd", (128, NFF, D), BF16))
                wpg = T(sb(st, "g_wpg", (128, 8, D), BF16))
                wpp = T(sb(st, "g_wpp", (128, 2, D), BF16))
                gp = T(sb(st, "g_gp", (128, 8), F32))
                gfin = T(sb(st, "g_gfin", (128, 8), F32))
                atr = Ring([sb(st, "g_at%d" % i, (128, NFF, 512), BF16) for i in range(1)])
                hin = T(sb(st, "g_hin", (128, 8, 512), F32))
                h2 = T(sb(st, "g_h2", (128, 8, 512), F32))
                h3 = T(sb(st, "g_h3", (128, 8, 512), F32))
                sqt = T(sb(st, "g_sq", (128, 8, 512), BF16))
                hp = T(sb(st, "g_hp", (128, 8, 512), BF16))
                rs = T(sb(st, "g_rs", (128, 512), F32))
                ptl = Ring([sb(st, "g_pt%d" % i, (128, 2, 512), BF16) for i in range(1)])
                sgr = Ring([sb(st, "g_sg%d" % i, (128, 512), F32) for i in range(2)])
                pss = T(ps(st, "g_pss"))
                psr = Ring([ps(st, "g_ps%d" % i) for i in range(3)])
                psg = Ring([ps(st, "g_psg%d" % i) for i in range(2)])
                psp = Ring([ps(st, "g_psp%d" % i) for i in range(2)])
                wst = Ring([sb(st, "g_wst%d" % i, (128, 1024), F32) for i in range(2)])
                wload(wd, W["w_down"][L], NFF, wst)
                wload(wpg, W["w_ple_gate"][L], 8, wst)
                wload(wpp, W["w_ple_proj"][L], 2, wst)
                pstg = Ring([sb(st, "g_pstg%d" % i, (128, 2, 512), F32) for i in range(1)])
                em.dma("sp", gp.t[:], W["g_ple"][L], writes=[gp])
                em.dma("sp", gfin.t[:], W["g_fin"], writes=[gfin])
                em.flush()
                for g in range(NG):
                    t0 = g * 512
                    at, pt = atr.next(), ptl.next()
                    em.dma("sp", at.t[:], hview(actT, t0), writes=[at])
                    em.dma("sp", hin.t[:], hview(hT, t0), writes=[hin])
                    pq = pstg.next()
                    em.dma("sp", pq.t[:], pT[L].rearrange("(c p) t -> p c t", p=128)[:, :, t0:t0 + 512], writes=[pq])
                    em.op("pool", lambda e, pq=pq, pt=pt: e.tensor_copy(pt.t[:], pq.t[:]), reads=[pq], writes=[pt])
                    for fc in range(8):
                        p = psr.next()
                        for f in range(NFF):
                            em.op("pe", lambda e, p=p, f=f, fc=fc, at=at: e.matmul(
                                p.t[:], wd.t[:, f, fc * 128:(fc + 1) * 128], at.t[:, f, :], start=(f == 0), stop=(f == NFF - 1)),
                                reads=[wd, at], writes=[p])
                        em.op("dve", lambda e, p=p, fc=fc: e.tensor_tensor(h2.t[:, fc, :], p.t[:], hin.t[:, fc, :], ALU.add),
                              reads=[p, hin], writes=[h2])
                    fm_norm(h2, gp, 8, sqt, pss, rs, 1.0 / D, EPS, hn=hp)
                    for fc in range(8):
                        pg, pp_ = psg.next(), psp.next()
                        for c_ in range(8):
                            em.op("pe", lambda e, pg=pg, c_=c_, fc=fc: e.matmul(
                                pg.t[:], wpg.t[:, c_, fc * 128:(fc + 1) * 128], hp.t[:, c_, :], start=(c_ == 0), stop=(c_ == 7)),
                                reads=[wpg, hp], writes=[pg])
                        for j in range(2):
                            em.op("pe", lambda e, pp_=pp_, j=j, fc=fc, pt=pt: e.matmul(
                                pp_.t[:], wpp.t[:, j, fc * 128:(fc + 1) * 128], pt.t[:, j, :], start=(j == 0), stop=(j == 1)),
                                reads=[wpp, pt], writes=[pp_])
                        sg = sgr.next()
                        em.op("act", lambda e, pg=pg, sg=sg: e.activation(out=sg.t[:], in_=pg.t[:], func=AF.Sigmoid), reads=[pg], writes=[sg])
                        em.op("dve", lambda e, pp_=pp_, sg=sg: e.tensor_tensor(sg.t[:], sg.t[:], pp_.t[:], ALU.mult),
                              reads=[pp_, sg], writes=[sg])
                        em.op("pool", lambda e, sg=sg, fc=fc: e.tensor_tensor(h3.t[:, fc, :], sg.t[:], h2.t[:, fc, :], ALU.add),
                              reads=[sg, h2], writes=[h3])
                    if not last:
                        em.dma("sp", hview(hT, t0), h3.t[:], reads=[h3])
                    else:
                        fm_norm(h3, gfin, 8, sqt, pss, rs, 1.0 / D, EPS, hn=None)
                        for c_ in range(8):
                            em.op("dve", lambda e, c_=c_: e.scalar_tensor_tensor(
                                h2.t[:, c_, :], h3.t[:, c_, :], gfin.t[:, c_:c_ + 1], rs.t[:], ALU.mult, ALU.mult),
                                reads=[h3, gfin, rs], writes=[h2])
                        em.dma("sp", hview(yT, t0), h2.t[:], reads=[h2])
                em.flush()
    return nc, em.nins


def run(inp, S, depth, nb, debug=False, upto=99):
    w = prep_weights({k: np.asarray(v, np.float32) for k, v in inp.items() if k not in ("x", "p")})
    consts = make_consts(S)
    nc, nins = build(S, depth, debug, upto)
    x = np.asarray(inp["x"], np.float32)
    p = np.asarray(inp["p"], np.float32)
    in_maps = []
    for b in range(nb):
        m = {"xT": np.ascontiguousarray(x[b].T), "pT": np.ascontiguousarray(p[:, b].transpose(0, 2, 1))}
        m.update(w)
        for k, v in consts.items():
            m["c_" + k] = v
        in_maps.append(m)
    res = run_bass_kernel_spmd(nc, in_maps, core_ids=list(range(nb)))
    out = np.stack([np.ascontiguousarray(res.results[b]["yT"].T) for b in range(nb)], 0).astype(np.float32)
    return out, res


def kernel(**inputs):
    out, _ = run(inputs, SEQ, DEPTH, BATCH)
    return out
```

```python
import math
import os
import numpy as np
import ml_dtypes
import concourse.bass as bass
import concourse.mybir as mybir
from concourse.bass_utils import run_bass_kernel_spmd

F32 = mybir.dt.float32
BF16 = mybir.dt.bfloat16
AF = mybir.ActivationFunctionType
ALU = mybir.AluOpType
AX = mybir.AxisListType

D = 1024
DEPTH = 4
SEQ = 8192
BATCH = 4
D_IN = 2664
D_FF = 2816
NFF = D_FF // 128
EPS = 1e-6
NIT = 16
TOPK = 256
NEG = -1.0e30

O_CQ, O_CKV, O_KR, O_DQ, O_DK, O_DV, O_IQ, O_IK, O_IW, O_SQ, O_SK, O_SV = (
    0, 384, 640, 672, 992, 1056, 1120, 1632, 1696, 1704, 2024, 2344)
M_MLA, M_DSA, M_SB = 0, 384, 704

STRICT_SAME_ENGINE = os.environ.get("STRICT", "1") == "1"


class Ins:
    __slots__ = ("eng", "fn", "deps", "inc", "sem", "val", "epoch", "isdma", "slotwait")


class Buf:
    __slots__ = ("w", "r", "rd")

    def __init__(self):
        self.w = None
        self.r = {}
        self.rd = []


class T:
    def __init__(self, t):
        self.t = t
        self.b = Buf()


class Em:
    ENG = ("pe", "act", "dve", "pool", "sp")
    BLK = {"pe": "tensor", "act": "scalar", "dve": "vector", "pool": "gpsimd", "sp": "sync"}

    def __init__(self, nc, csems, dsems):
        self.nc = nc
        self.csem = csems
        self.dsem = dsems
        self.cnt = {e: 0 for e in csems}
        self.dcount = {q: 0 for q in dsems}
        self.dlast = {q: [None] * len(dsems[q]) for q in dsems}
        self.ops = {e: [] for e in self.ENG}
        self.seen = {e: {} for e in self.ENG}
        self.epoch = 0
        self.nins = 0

    def _mk(self, eng, fn, reads, writes, isdma):
        ins = Ins()
        ins.eng, ins.fn, ins.isdma, ins.inc, ins.epoch = eng, fn, isdma, False, self.epoch
        ins.sem = ins.val = ins.slotwait = None
        deps = set()
        for b in reads:
            if b.w is not None:
                deps.add(b.w)
        for b in writes:
            if b.w is not None:
                deps.add(b.w)
            deps.update(b.r.values())
            deps.update(b.rd)
        for b in reads:
            if isdma:
                b.rd.append(ins)
            else:
                b.r[eng] = ins
        for b in writes:
            b.w = ins
            b.r = {}
            b.rd = []
        out = []
        for d in deps:
            if d is ins or d.epoch != self.epoch:
                continue
            if (not isdma) and (not d.isdma) and d.eng == eng:
                if eng == "pe" or not STRICT_SAME_ENGINE:
                    continue
            out.append(d)
        ins.deps = out
        self.ops[eng].append(ins)
        self.nins += 1
        return ins

    def op(self, eng, fn, reads=(), writes=()):
        return self._mk(eng, fn, [x.b if isinstance(x, T) else x for x in reads],
                        [x.b if isinstance(x, T) else x for x in writes], False)

    def dma(self, q, out, in_, reads=(), writes=()):
        return self._mk(q, lambda e: e.dma_start(out=out, in_=in_),
                        [x.b if isinstance(x, T) else x for x in reads],
                        [x.b if isinstance(x, T) else x for x in writes], True)

    def flush(self):
        for e in self.ENG:
            for ins in self.ops[e]:
                for d in ins.deps:
                    d.inc = True
        for e in self.ENG:
            for ins in self.ops[e]:
                if ins.isdma:
                    n = len(self.dsem[e])
                    k = self.dcount[e]
                    self.dcount[e] += 1
                    slot = k % n
                    ins.sem = self.dsem[e][slot]
                    ins.val = 16 * (k // n + 1)
                    prev = self.dlast[e][slot]
                    if prev is not None and prev.epoch == self.epoch:
                        ins.slotwait = (prev.sem, prev.val)
                    self.dlast[e][slot] = ins
                elif ins.inc:
                    self.cnt[e] += 1
                    ins.sem = self.csem[e]
                    ins.val = self.cnt[e]
        finals = []
        for q in self.dsem:
            n = len(self.dsem[q])
            for slot in range(n):
                last = self.dlast[q][slot]
                if last is not None and last.epoch == self.epoch:
                    finals.append((last.sem, last.val))
        with self.nc.Block() as block:
            for e in self.ENG:
                ops = self.ops[e]
                seen = self.seen[e]

                def body(q, ops=ops, seen=seen, e=e):
                    for ins in ops:
                        waits = {}
                        for d in ins.deps:
                            key = id(d.sem)
                            if key not in waits or waits[key][1] < d.val:
                                waits[key] = (d.sem, d.val)
                        if ins.slotwait is not None:
                            key = id(ins.slotwait[0])
                            if key not in waits or waits[key][1] < ins.slotwait[1]:
                                waits[key] = ins.slotwait
                        for key, (sem, val) in waits.items():
                            if seen.get(key, 0) < val:
                                q.wait_ge(sem, val)
                                seen[key] = val
                        r = ins.fn(q)
                        if ins.isdma:
                            r.then_inc(ins.sem, 16)
                        elif ins.inc:
                            r.then_inc(ins.sem, 1)
                    if e == "sp":
                        for sem, val in finals:
                            if seen.get(id(sem), 0) < val:
                                q.wait_ge(sem, val)
                                seen[id(sem)] = val
                if ops or e == "sp":
                    getattr(block, self.BLK[e])(body)
        self.ops = {e: [] for e in self.ENG}
        self.epoch += 1


class Ring:
    def __init__(self, tiles):
        self.tiles = [T(t) for t in tiles]
        self.i = 0

    def next(self):
        t = self.tiles[self.i % len(self.tiles)]
        self.i += 1
        return t


def t5_bucket_np(dist):
    max_exact = 16
    d = np.maximum(dist, 1).astype(np.float32)
    large = max_exact + (np.log(d / np.float32(max_exact)) / np.float32(math.log(128 / max_exact))
                         * np.float32(32 - max_exact)).astype(np.int32)
    large = np.minimum(large, 31)
    return np.where(dist < max_exact, dist, large)


def make_consts(S):
    c = {}
    half = 16
    inv = (10000.0 ** (-np.arange(half, dtype=np.float32) / half)).astype(np.float32)
    ang = np.arange(S, dtype=np.float32)[:, None] * inv[None, :]
    cos = np.cos(ang).astype(np.float32).T
    sin = np.sin(ang).astype(np.float32).T
    c["cosT"] = np.ascontiguousarray(np.concatenate([cos, cos], 0))
    c["sinT"] = np.ascontiguousarray(np.concatenate([-sin, sin], 0))
    k = np.arange(128)[:, None]
    q = np.arange(512)[None, :]
    mi = np.zeros((4, 128, 512), np.float32)
    ms = np.zeros((4, 128, 512), np.float32)
    for kb in range(4):
        mi[kb] = ((kb * 128 + k) <= q)
        ms[kb] = ((kb * 128 + k) < q)
    c["mask_incl"] = mi.astype(ml_dtypes.bfloat16)
    c["mask_strict"] = ms.astype(ml_dtypes.bfloat16)
    j = np.arange(128)
    c["tri"] = (j[:, None] >= j[None, :]).astype(np.float32).astype(ml_dtypes.bfloat16)
    c["ones"] = np.ones((128, 128), ml_dtypes.bfloat16)
    c["ident"] = np.eye(128, dtype=np.float32).astype(ml_dtypes.bfloat16)
    sel = np.zeros((128, 64), np.float32)
    sel[64, :] = 1.0
    c["sel"] = sel
    oh = np.zeros((32, 128, 2, 128), np.float32)
    kk = np.arange(128)[:, None]
    qq = np.arange(128)[None, :]
    for dl in range(2):
        dist = np.maximum(128 * dl + qq - kk, 0)
        bk = t5_bucket_np(dist)
        for b in range(32):
            oh[b, :, dl, :] = (bk == b)
    c["oh"] = oh
    qi = np.arange(128)[:, None]
    ki = np.arange(128)[None, :]
    c["causal_add"] = np.where(ki <= qi, 0.0, NEG).astype(np.float32)
    c["pow2"] = np.tile((0.5 ** np.arange(1, NIT + 1)).astype(np.float32)[None, :], (128, 1))
    return c


CONST_SPECS = [("cosT", None, F32), ("sinT", None, F32), ("mask_incl", (4, 128, 512), BF16),
               ("mask_strict", (4, 128, 512), BF16), ("tri", (128, 128), BF16), ("ones", (128, 128), BF16),
               ("ident", (128, 128), BF16), ("sel", (128, 64), F32), ("oh", (32, 128, 2, 128), F32),
               ("causal_add", (128, 128), F32), ("pow2", (128, NIT), F32)]


def prep_weights(inp):
    w = {}
    w_in = inp["w_in"]
    L = w_in.shape[0]
    w["w_in"] = np.ascontiguousarray(w_in)
    w["w_tok"] = np.ascontiguousarray(np.concatenate(
        [w_in[:, :, O_DV:O_DV + 64], w_in[:, :, O_SV:O_SV + 320], w_in[:, :, O_IW:O_IW + 8]], axis=2))
    wkr = np.zeros((L, D, 2, 96), np.float32)
    wkr[:, :, 0, 64:96] = w_in[:, :, O_KR:O_KR + 32]
    wkr[:, :, 1, 64:80] = w_in[:, :, O_KR + 16:O_KR + 32]
    wkr[:, :, 1, 80:96] = w_in[:, :, O_KR:O_KR + 16]
    w["w_kr"] = wkr
    wuq = inp["mla_w_uq"]
    w["w_uq"] = np.ascontiguousarray(wuq)
    sw = wuq.reshape(L, 384, 6, 96).copy()
    sw[:, :, :, 64:80] = wuq.reshape(L, 384, 6, 96)[:, :, :, 80:96]
    sw[:, :, :, 80:96] = wuq.reshape(L, 384, 6, 96)[:, :, :, 64:80]
    w["w_uqs"] = np.ascontiguousarray(sw.reshape(L, 384, 576))
    wukv = inp["mla_w_ukv"].reshape(L, 256, 6, 128)
    w["w_uk"] = np.ascontiguousarray(wukv[:, :, :, :64].reshape(L, 256, 384))
    w["w_uv"] = np.ascontiguousarray(wukv[:, :, :, 64:].reshape(L, 256, 384))
    g = lambda a, n: np.ascontiguousarray(a.reshape(L, n, 128).transpose(0, 2, 1))
    w["g_attn"] = g(inp["attn_norm"], 8)
    w["g_ffn"] = g(inp["ffn_norm"], 8)
    w["g_ple"] = g(inp["ple_norm"], 8)
    w["g_q"] = g(inp["mla_q_norm"], 3)
    w["g_kv"] = g(inp["mla_kv_norm"], 2)
    w["g_fin"] = np.ascontiguousarray(inp["final_norm"].reshape(8, 128).T)
    for k in ("w_o", "w_gate", "w_up", "w_down", "w_ple_gate", "w_ple_proj", "rel_bias"):
        w[k] = np.ascontiguousarray(inp[k])
    return w


def build(S, depth, debug=False, upto=99):
    NG = S // 512
    NBLK = S // 128
    nc = bass.Bass("TRN2", target_bir_lowering=False)
    dt_in = lambda name, shape, dt=F32: nc.dram_tensor(name, list(shape), dt, kind="ExternalInput").ap()
    dkind = "ExternalOutput" if debug else "Internal"
    dt_sc = lambda name, shape, dt: nc.dram_tensor(name, list(shape), dt, kind=dkind).ap()

    xT = dt_in("xT", (D, S))
    pT = dt_in("pT", (depth, 256, S))
    W = {}
    for name, shape in [("w_in", (depth, D, D_IN)), ("w_tok", (depth, D, 392)), ("w_kr", (depth, D, 2, 96)),
                        ("w_uq", (depth, 384, 576)), ("w_uqs", (depth, 384, 576)), ("w_uk", (depth, 256, 384)),
                        ("w_uv", (depth, 256, 384)), ("g_attn", (depth, 128, 8)), ("g_ffn", (depth, 128, 8)),
                        ("g_ple", (depth, 128, 8)), ("g_q", (depth, 128, 3)), ("g_kv", (depth, 128, 2)),
                        ("g_fin", (128, 8)), ("w_o", (depth, D, D)), ("w_gate", (depth, D, D_FF)),
                        ("w_up", (depth, D, D_FF)), ("w_down", (depth, D_FF, D)), ("w_ple_gate", (depth, D, D)),
                        ("w_ple_proj", (depth, 256, D)), ("rel_bias", (32, 5))]:
        W[name] = dt_in(name, shape)
    C = {}
    for name, shape, dt in CONST_SPECS:
        if shape is None:
            shape = (32, S)
        C[name] = dt_in("c_" + name, shape, dt)
    yT = nc.dram_tensor("yT", [D, S], F32, kind="ExternalOutput").ap()

    hT = dt_sc("hT", (D, S), F32)
    projT = dt_sc("projT", (D_IN, S), BF16)
    mlaQT = dt_sc("mlaQT", (6, 96, S), BF16)
    mlaKT = dt_sc("mlaKT", (6, 96, S), BF16)
    mlaV = dt_sc("mlaV", (6, 128, NBLK, 66), BF16)
    dsaV = dt_sc("dsaV", (128, NBLK, 66), BF16)
    sbV = dt_sc("sbV", (5, 128, NBLK, 64), BF16)
    idxW = dt_sc("idxW", (NBLK, 128, 8), F32)
    mixT = dt_sc("mixT", (D, S), BF16)
    actT = dt_sc("actT", (D_FF, S), BF16)

    from contextlib import ExitStack
    top = ExitStack()
    with top:
        NSP, NPQ = 16, 8
        csems = {e: top.enter_context(nc.semaphore("s_" + e)) for e in ("pe", "act", "dve", "pool")}
        dsems = {"sp": [top.enter_context(nc.semaphore("d_sp%d" % i)) for i in range(NSP)]}
        em = Em(nc, csems, dsems)

        uid = [0]

        def sb(stack, name, shape, dt):
            uid[0] += 1
            return stack.enter_context(nc.sbuf_tensor("%s_%d" % (name, uid[0]), list(shape), dt))

        def ps(stack, name, shape=(128, 512), dt=F32):
            uid[0] += 1
            return stack.enter_context(nc.psum_tensor("%s_%d" % (name, uid[0]), list(shape), dt))

        ones = T(sb(top, "k_ones", (128, 128), BF16))
        tri = T(sb(top, "k_tri", (128, 128), BF16))
        ident = T(sb(top, "k_ident", (128, 128), BF16))
        sel = T(sb(top, "k_sel", (128, 64), F32))
        mincl = T(sb(top, "k_mincl", (128, 4, 512), BF16))
        mstr = T(sb(top, "k_mstr", (128, 4, 512), BF16))
        cadd = T(sb(top, "k_cadd", (128, 128), F32))
        pow2 = T(sb(top, "k_pow2", (128, NIT), F32))
        ctab = T(sb(top, "k_ctab", (128, 2, 5, 128), BF16))
        b31 = T(sb(top, "k_b31", (128, 5), F32))

        with ExitStack() as st:
            oh = T(sb(st, "s_oh", (128, 32, 256), F32))
            rbb = T(sb(st, "s_rbb", (128, 160), F32))
            dcol = T(sb(st, "s_dcol", (128, 32, 5), F32))
            acc = T(sb(st, "s_acc", (128, 5, 256), F32))
            em.dma("sp", ones.t[:], C["ones"], writes=[ones])
            em.dma("sp", tri.t[:], C["tri"], writes=[tri])
            em.dma("sp", ident.t[:], C["ident"], writes=[ident])
            em.dma("sp", sel.t[:], C["sel"], writes=[sel])
            em.dma("sp", mincl.t[:], C["mask_incl"].rearrange("a p q -> p a q"), writes=[mincl])
            em.dma("sp", mstr.t[:], C["mask_strict"].rearrange("a p q -> p a q"), writes=[mstr])
            em.dma("sp", cadd.t[:], C["causal_add"], writes=[cadd])
            em.dma("sp", pow2.t[:], C["pow2"], writes=[pow2])
            for b in range(32):
                em.dma("sp", oh.t[:, b, :], C["oh"][b].rearrange("p a q -> p (a q)"), writes=[oh])
            rb_flat = W["rel_bias"].rearrange("b h -> (b h)")
            em.dma("sp", rbb.t[:], bass.AP(rb_flat.tensor, rb_flat.offset, [[0, 128], [1, 160]]), writes=[rbb])
            rb3 = rbb.t[:].rearrange("p (b h) -> p b h", h=5)
            em.op("dve", lambda e: e.tensor_copy(b31.t[:], rb3[:, 31, :]), reads=[rbb], writes=[b31])
            for h in range(5):
                em.op("dve", lambda e, h=h: e.tensor_scalar(dcol.t[:, :, h], rb3[:, :, h], b31.t[:, h:h + 1], None,
                                                            ALU.subtract), reads=[rbb, b31], writes=[dcol])
            for h in range(5):
                for b in range(31):
                    if b == 0:
                        em.op("dve", lambda e, h=h, b=b: e.tensor_scalar(
                            acc.t[:, h, :], oh.t[:, b, :], dcol.t[:, b, h:h + 1], None, ALU.mult),
                            reads=[oh, dcol], writes=[acc])
                    else:
                        em.op("dve", lambda e, h=h, b=b: e.scalar_tensor_tensor(
                            acc.t[:, h, :], oh.t[:, b, :], dcol.t[:, b, h:h + 1], acc.t[:, h, :], ALU.mult, ALU.add),
                            reads=[oh, dcol, acc], writes=[acc])
            for h in range(5):
                for dl in range(2):
                    em.op("act", lambda e, h=h, dl=dl: e.activation(
                        out=ctab.t[:, dl, h, :], in_=acc.t[:, h, dl * 128:(dl + 1) * 128], func=AF.Exp),
                        reads=[acc], writes=[ctab])
            em.flush()
        if upto < 1:
            return nc, em.nins

        WST = 1408

        def wload(dst, src, nchunk, wst):
            v = src.rearrange("(c p) n -> p c n", p=128)
            n = v.shape[2]
            for c_ in range(nchunk):
                for n0 in range(0, n, WST):
                    n1 = min(n, n0 + WST)
                    s_ = wst.next()
                    em.dma("sp", s_.t[:, 0:n1 - n0], v[:, c_, n0:n1], writes=[s_])
                    em.op("pool", lambda e, s_=s_, c_=c_, n0=n0, n1=n1: e.tensor_copy(dst.t[:, c_, n0:n1], s_.t[:, 0:n1 - n0]),
                          reads=[s_], writes=[dst])

        def fm_norm(hin, gain, nchunk, sqt, psb, rs, scale, eps, hn=None, out_f32=None):
            em.op("act", lambda e: e.activation(out=sqt.t[:, 0:nchunk, :], in_=hin.t[:, 0:nchunk, :], func=AF.Square),
                  reads=[hin], writes=[sqt])
            for c_ in range(nchunk):
                em.op("pe", lambda e, c_=c_: e.matmul(psb.t[:], ones.t[:], sqt.t[:, c_, :],
                                                      start=(c_ == 0), stop=(c_ == nchunk - 1)),
                      reads=[ones, sqt], writes=[psb])
            em.op("dve", lambda e: e.tensor_scalar(rs.t[:], psb.t[:], scale, eps, ALU.mult, ALU.add),
                  reads=[psb], writes=[rs])
            em.op("act", lambda e: e.activation(out=rs.t[:], in_=rs.t[:], func=AF.Sqrt), reads=[rs], writes=[rs])
            em.op("dve", lambda e: e.reciprocal(rs.t[:], rs.t[:]), reads=[rs], writes=[rs])
            if hn is not None:
                for c_ in range(nchunk):
                    em.op("dve", lambda e, c_=c_: e.scalar_tensor_tensor(
                        hn.t[:, c_, :], hin.t[:, c_, :], gain.t[:, c_:c_ + 1], rs.t[:], ALU.mult, ALU.mult),
                        reads=[hin, gain, rs], writes=[hn])

        hview = lambda ap, t0, n=512: ap.rearrange("(c p) t -> p c t", p=128)[:, :, t0:t0 + n]

        for L in range(depth):
            hsrc = xT if L == 0 else hT
            with ExitStack() as st:
                win = T(sb(st, "a_win", (128, 8, D_IN), BF16))
                wtok = T(sb(st, "a_wtok", (128, 8, 392), BF16))
                wkr = T(sb(st, "a_wkr", (128, 8, 192), BF16))
                wuq = T(sb(st, "a_wuq", (128, 3, 576), BF16))
                wuqs = T(sb(st, "a_wuqs", (128, 3, 576), BF16))
                wuk = T(sb(st, "a_wuk", (128, 2, 384), BF16))
                wuv = T(sb(st, "a_wuv", (128, 2, 384), BF16))
                gat = T(sb(st, "a_gat", (128, 8), F32))
                gq = T(sb(st, "a_gq", (128, 3), F32))
                gkv = T(sb(st, "a_gkv", (128, 2), F32))
                hin = T(sb(st, "a_hin", (128, 8, 512), F32))
                sqt = T(sb(st, "a_sq", (128, 8, 512), BF16))
                hn = T(sb(st, "a_hn", (128, 8, 512), BF16))
                rs = T(sb(st, "a_rs", (128, 512), F32))
                rq = T(sb(st, "a_rq", (128, 512), F32))
                rkv = T(sb(st, "a_rkv", (128, 512), F32))
                cs = T(sb(st, "a_cs", (128, 2, 512), F32))
                raw = T(sb(st, "a_raw", (128, 5, 512), F32))
                sq2 = T(sb(st, "a_sq2", (128, 5, 512), BF16))
                cqn = T(sb(st, "a_cqn", (128, 3, 512), BF16))
                ckvn = T(sb(st, "a_ckvn", (128, 2, 512), BF16))
                stg = Ring([sb(st, "a_stg%d" % i, (128, 512), BF16) for i in range(4)])
                t1r = Ring([sb(st, "a_t1%d" % i, (128, 512), F32) for i in range(2)])
                t2r = Ring([sb(st, "a_t2%d" % i, (128, 512), F32) for i in range(2)])
                dvt = T(sb(st, "a_dvt", (128, 4, 66), BF16))
                svt = T(sb(st, "a_svt", (128, 4, 320), BF16))
                iwt = T(sb(st, "a_iwt", (128, 4, 8), F32))
                mvt = T(sb(st, "a_mvt", (128, 4, 6, 66), BF16))
                psr = Ring([ps(st, "a_ps%d" % i) for i in range(6)])
                pss = T(ps(st, "a_pss"))
                pss2 = T(ps(st, "a_pss2"))

                ABITS0 = int(os.environ.get('A_BITS', '63'))
                if not (ABITS0 & 64):
                    wst = Ring([sb(st, "a_wst%d" % i, (128, WST), F32) for i in range(2)])
                    wload(win, W["w_in"][L], 8, wst)
                    wload(wtok, W["w_tok"][L], 8, wst)
                    wload(wkr, W["w_kr"][L].rearrange("d a m -> d (a m)"), 8, wst)
                    wload(wuq, W["w_uq"][L], 3, wst)
                    wload(wuqs, W["w_uqs"][L], 3, wst)
                    wload(wuk, W["w_uk"][L], 2, wst)
                    wload(wuv, W["w_uv"][L], 2, wst)
                em.dma("sp", gat.t[:], W["g_attn"][L], writes=[gat])
                em.dma("sp", gq.t[:], W["g_q"][L], writes=[gq])
                em.dma("sp", gkv.t[:], W["g_kv"][L], writes=[gkv])
                em.op("pool", lambda e: e.memset(dvt.t[:], 1.0), writes=[dvt])
                em.op("pool", lambda e: e.memset(mvt.t[:], 1.0), writes=[mvt])
                if os.environ.get("SPLITW", "1") == "1":
                    em.flush()
                evq = [0]

                def evac(dst_ap, src_ap, reads, writes, scale=None):
                    evq[0] += 1
                    if evq[0] % 2 == 0:
                        if scale is None:
                            em.op("act", lambda e: e.copy(dst_ap, src_ap), reads=reads, writes=writes)
                        else:
                            em.op("act", lambda e: e.mul(dst_ap, src_ap, scale), reads=reads, writes=writes)
                    else:
                        if scale is None:
                            em.op("dve", lambda e: e.tensor_copy(dst_ap, src_ap), reads=reads, writes=writes)
                        else:
                            em.op("dve", lambda e: e.tensor_scalar(dst_ap, src_ap, scale, None, ALU.mult),
                                  reads=reads, writes=writes)

                apasses = [int(x) for x in os.environ.get('A_PASSES', '63').split(',')]
                for (ABITS, g) in [(ab, g_) for ab in apasses for g_ in range(NG)]:
                    if g == 0 and ABITS != apasses[0]:
                        em.flush()
                    t0 = g * 512
                    em.dma("sp", hin.t[:], hview(hsrc, t0), writes=[hin])
                    em.dma("sp", cs.t[64:96, 0, :], C["cosT"][:, t0:t0 + 512], writes=[cs])
                    em.dma("sp", cs.t[64:96, 1, :], C["sinT"][:, t0:t0 + 512], writes=[cs])
                    fm_norm(hin, gat, 8, sqt, pss, rs, 1.0 / D, EPS, hn=hn)
                    for (c0, n, scale) in [] if not (ABITS & 1) else [(O_DQ, 320, 0.125), (O_DK, 64, None), (O_IQ, 512, None), (O_IK, 64, None),
                                           (O_SQ, 320, 0.125), (O_SK, 320, None)]:
                        m0 = 0
                        while m0 < n:
                            M = min(128, n - m0)
                            p = psr.next()
                            for c_ in range(8):
                                em.op("pe", lambda e, p=p, c_=c_, a=c0 + m0, M=M: e.matmul(
                                    p.t[0:M, :], win.t[:, c_, a:a + M], hn.t[:, c_, :], start=(c_ == 0), stop=(c_ == 7)),
                                    reads=[win, hn], writes=[p])
                            o = stg.next()
                            evac(o.t[0:M, :], p.t[0:M, :], [p], [o], scale)
                            em.dma("sp", projT[c0 + m0:c0 + m0 + M, t0:t0 + 512], o.t[0:M, :], reads=[o])
                            m0 += M
                    if not (ABITS & 2):
                        continue
                    for j in range(5):
                        p = psr.next()
                        for c_ in range(8):
                            em.op("pe", lambda e, p=p, c_=c_, j=j: e.matmul(
                                p.t[:], win.t[:, c_, j * 128:(j + 1) * 128], hn.t[:, c_, :], start=(c_ == 0), stop=(c_ == 7)),
                                reads=[win, hn], writes=[p])
                        evac(raw.t[:, j, :], p.t[:], [p], [raw])
                    em.op("act", lambda e: e.activation(out=sq2.t[:], in_=raw.t[:], func=AF.Square), reads=[raw], writes=[sq2])
                    for j in range(3):
                        em.op("pe", lambda e, j=j: e.matmul(pss.t[:], ones.t[:], sq2.t[:, j, :], start=(j == 0), stop=(j == 2)),
                              reads=[ones, sq2], writes=[pss])
                    for j in range(2):
                        em.op("pe", lambda e, j=j: e.matmul(pss2.t[:], ones.t[:], sq2.t[:, 3 + j, :], start=(j == 0), stop=(j == 1)),
                              reads=[ones, sq2], writes=[pss2])
                    for (r_, p_, sc_, ep_) in [(rq, pss, 96.0 / 384.0, 96.0 * EPS), (rkv, pss2, 1.0 / 256.0, EPS)]:
                        em.op("dve", lambda e, r_=r_, p_=p_, sc_=sc_, ep_=ep_: e.tensor_scalar(
                            r_.t[:], p_.t[:], sc_, ep_, ALU.mult, ALU.add), reads=[p_], writes=[r_])
                        em.op("act", lambda e, r_=r_: e.activation(out=r_.t[:], in_=r_.t[:], func=AF.Sqrt), reads=[r_], writes=[r_])
                        em.op("dve", lambda e, r_=r_: e.reciprocal(r_.t[:], r_.t[:]), reads=[r_], writes=[r_])
                    for j in range(3):
                        em.op("dve", lambda e, j=j: e.scalar_tensor_tensor(
                            cqn.t[:, j, :], raw.t[:, j, :], gq.t[:, j:j + 1], rq.t[:], ALU.mult, ALU.mult),
                            reads=[raw, gq, rq], writes=[cqn])
                    for j in range(2):
                        em.op("dve", lambda e, j=j: e.scalar_tensor_tensor(
                            ckvn.t[:, j, :], raw.t[:, 3 + j, :], gkv.t[:, j:j + 1], rkv.t[:], ALU.mult, ALU.mult),
                            reads=[raw, gkv, rkv], writes=[ckvn])

                    def rope_out(pa, pb, o):
                        a_, b_ = t1r.next(), t2r.next()
                        em.op("dve", lambda e: e.tensor_tensor(a_.t[64:96, :], pa.t[64:96, :], cs.t[64:96, 0, :], ALU.mult),
                              reads=[pa, cs], writes=[a_])
                        em.op("dve", lambda e: e.tensor_tensor(b_.t[64:96, :], pb.t[64:96, :], cs.t[64:96, 1, :], ALU.mult),
                              reads=[pb, cs], writes=[b_])
                        em.op("pool", lambda e: e.tensor_tensor(o.t[64:96, :], a_.t[64:96, :], b_.t[64:96, :], ALU.add),
                              reads=[a_, b_], writes=[o])

                    for h in range(6 if (ABITS & 4) else 0):
                        pa, pb = psr.next(), psr.next()
                        for (p, w_) in [(pa, wuq), (pb, wuqs)]:
                            for j in range(3):
                                em.op("pe", lambda e, p=p, w_=w_, j=j, h=h: e.matmul(
                                    p.t[0:96, :], w_.t[:, j, h * 96:(h + 1) * 96], cqn.t[:, j, :], start=(j == 0), stop=(j == 2)),
                                    reads=[w_, cqn], writes=[p])
                        o = stg.next()
                        em.op("act", lambda e, o=o, pa=pa: e.copy(o.t[0:64, :], pa.t[0:64, :]), reads=[pa], writes=[o])
                        rope_out(pa, pb, o)
                        em.dma("sp", mlaQT[h, :, t0:t0 + 512], o.t[0:96, :], reads=[o])
                    for h in range(6 if (ABITS & 8) else 0):
                        p = psr.next()
                        for j in range(2):
                            em.op("pe", lambda e, p=p, j=j, h=h: e.matmul(
                                p.t[0:64, :], wuk.t[:, j, h * 64:(h + 1) * 64], ckvn.t[:, j, :], start=(j == 0), stop=(j == 1)),
                                reads=[wuk, ckvn], writes=[p])
                        o = stg.next()
                        evac(o.t[0:64, :], p.t[0:64, :], [p], [o])
                        em.dma("sp", mlaKT[h, 0:64, t0:t0 + 512], o.t[0:64, :], reads=[o])
                    if ABITS & 16:
                        pa, pb = psr.next(), psr.next()
                        for (p, v_) in [(pa, 0), (pb, 1)]:
                            for c_ in range(8):
                                em.op("pe", lambda e, p=p, v_=v_, c_=c_: e.matmul(
                                    p.t[0:96, :], wkr.t[:, c_, v_ * 96:(v_ + 1) * 96], hn.t[:, c_, :], start=(c_ == 0), stop=(c_ == 7)),
                                    reads=[wkr, hn], writes=[p])
                        o = stg.next()
                        rope_out(pa, pb, o)
                        for h in range(6):
                            em.dma("sp", mlaKT[h, 64:96, t0:t0 + 512], o.t[64:96, :], reads=[o])
                    if not (ABITS & 32):
                        continue
                    for blk in range(4):
                        if not (ABITS & 4096):
                            p = psr.next()
                            for c_ in range(8):
                                em.op("pe", lambda e, p=p, c_=c_, blk=blk: e.matmul(
                                    p.t[:, 0:392], hn.t[:, c_, blk * 128:(blk + 1) * 128], wtok.t[:, c_, :], start=(c_ == 0), stop=(c_ == 7)),
                                    reads=[wtok, hn], writes=[p])
                            if not (ABITS & 16384):
                                em.op("dve", lambda e, p=p, blk=blk: e.tensor_copy(dvt.t[:, blk, 0:64], p.t[:, 0:64]), reads=[p], writes=[dvt])
                            em.op("dve", lambda e, p=p, blk=blk: e.tensor_copy(svt.t[:, blk, :], p.t[:, 64:384]), reads=[p], writes=[svt])
                            if not (ABITS & 32768):
                                em.op("dve", lambda e, p=p, blk=blk: e.tensor_copy(iwt.t[:, blk, :], p.t[:, 384:392]), reads=[p], writes=[iwt])
                        if not (ABITS & 8192):
                            p2 = psr.next()
                            for j in range(2):
                                em.op("pe", lambda e, p2=p2, j=j, blk=blk: e.matmul(
                                    p2.t[:, 0:384], ckvn.t[:, j, blk * 128:(blk + 1) * 128], wuv.t[:, j, :], start=(j == 0), stop=(j == 1)),
                                    reads=[wuv, ckvn], writes=[p2])
                            em.op("dve", lambda e, p2=p2, blk=blk: e.tensor_copy(
                                mvt.t[:, blk, :, 0:64], p2.t[:, 0:384].rearrange("p (h d) -> p h d", d=64)), reads=[p2], writes=[mvt])
                    if not (ABITS & 256):
                        em.dma("sp", dsaV[:, 4 * g:4 * g + 4, :], dvt.t[:], reads=[dvt])
                    if not (ABITS & 512):
                        for h in range(5):
                            em.dma("sp", sbV[h, :, 4 * g:4 * g + 4, :], svt.t[:, :, h * 64:(h + 1) * 64], reads=[svt])
                    if not (ABITS & 1024):
                        for h in range(6):
                            em.dma("sp", mlaV[h, :, 4 * g:4 * g + 4, :], mvt.t[:, :, h, :], reads=[mvt])
                    if not (ABITS & 2048):
                        em.dma("sp", idxW.rearrange("b p h -> p b h")[:, 4 * g:4 * g + 4, :], iwt.t[:], reads=[iwt])
                em.flush()

            if upto < 2:
                return nc, em.nins
            with ExitStack() as st:
                ktr = Ring([sb(st, "m_kt%d" % i, (96, S), BF16) for i in range(2)])
                qtr = Ring([sb(st, "m_qt%d" % i, (96, S), BF16) for i in range(2)])
                vr = Ring([sb(st, "m_v%d" % i, (128, NBLK, 66), BF16) for i in range(2)])
                ptr_ = Ring([sb(st, "m_pt%d" % i, (128, 512), BF16) for i in range(3)])
                osb = T(sb(st, "m_osb", (128, 512), F32))
                rb_ = T(sb(st, "m_rb", (64, 512), F32))
                mo = Ring([sb(st, "m_mo%d" % i, (64, 512), BF16) for i in range(2)])
                pss_ = Ring([ps(st, "m_ps%d" % i) for i in range(3)])
                pso = T(ps(st, "m_pso"))
                psb_ = T(ps(st, "m_psb"))
                items = []
                for h in range(6):
                    for G in range(NG):
                        for kb in range(4 * (G + 1)):
                            items.append((h, G, kb))
                hstate = {}

                def mla_front(h, G, kb):
                    if G == 0 and kb == 0:
                        kt, qt, v = ktr.next(), qtr.next(), vr.next()
                        em.dma("sp", kt.t[:], mlaKT[h], writes=[kt])
                        em.dma("sp", qt.t[:], mlaQT[h], writes=[qt])
                        em.dma("sp", v.t[:], mlaV[h], writes=[v])
                        hstate[h] = (kt, qt, v)
                    kt, qt, v = hstate[h]
                    t0 = G * 512
                    p = pss_.next()
                    em.op("pe", lambda e, p=p, kt=kt, qt=qt, kb=kb, t0=t0: e.matmul(
                        p.t[:], kt.t[0:96, kb * 128:(kb + 1) * 128], qt.t[0:96, t0:t0 + 512], start=True, stop=True),
                        reads=[kt, qt], writes=[p])
                    pt = ptr_.next()
                    em.op("act", lambda e, p=p, pt=pt: e.activation(out=pt.t[:], in_=p.t[:], func=AF.Exp),
                          reads=[p], writes=[pt])
                    if kb >= 4 * G:
                        em.op("pool", lambda e, pt=pt, kb=kb, G=G: e.tensor_tensor(
                            pt.t[:], pt.t[:], mincl.t[:, kb - 4 * G, :], ALU.mult), reads=[pt, mincl], writes=[pt])
                    return pt

                def mla_back(h, G, kb, pt):
                    kt, qt, v = hstate[h]
                    t0 = G * 512
                    nkb = 4 * (G + 1)
                    em.op("pe", lambda e, pt=pt, v=v, kb=kb, nkb=nkb: e.matmul(
                        pso.t[0:65, :], v.t[:, kb, 0:65], pt.t[:], start=(kb == 0), stop=(kb == nkb - 1)),
                        reads=[v, pt], writes=[pso])
                    if kb == nkb - 1:
                        em.op("act", lambda e: e.copy(osb.t[0:65, :], pso.t[0:65, :]), reads=[pso], writes=[osb])
                        em.op("pe", lambda e: e.matmul(psb_.t[0:64, :], sel.t[0:65, :], osb.t[0:65, :], start=True, stop=True),
                              reads=[sel, osb], writes=[psb_])
                        em.op("dve", lambda e: e.reciprocal(rb_.t[:], psb_.t[0:64, :]), reads=[psb_], writes=[rb_])
                        m = mo.next()
                        em.op("dve", lambda e, m=m: e.tensor_tensor(m.t[:], osb.t[0:64, :], rb_.t[:], ALU.mult),
                              reads=[osb, rb_], writes=[m])
                        em.dma("sp", mixT[M_MLA + 64 * h:M_MLA + 64 * h + 64, t0:t0 + 512], m.t[:], reads=[m])

                cur = mla_front(*items[0])
                for i, it in enumerate(items):
                    nxt = mla_front(*items[i + 1]) if i + 1 < len(items) else None
                    mla_back(*it, cur)
                    cur = nxt
                em.flush()

            if upto < 3:
                return nc, em.nins
            with ExitStack() as st:
                ktr = Ring([sb(st, "s_kt%d" % i, (64, S), BF16) for i in range(2)])
                qtr = Ring([sb(st, "s_qt%d" % i, (64, S), BF16) for i in range(2)])
                nqr = Ring([sb(st, "s_nq%d" % i, (64, S), BF16) for i in range(2)])
                vr = Ring([sb(st, "s_v%d" % i, (128, NBLK, 64), BF16) for i in range(2)])
                er = Ring([sb(st, "s_e%d" % i, (128, 512), F32) for i in range(2)])
                spr = Ring([sb(st, "s_sp%d" % i, (128, 512), BF16) for i in range(3)])
                wr = Ring([sb(st, "s_w%d" % i, (128, 512), BF16) for i in range(3)])
                R = T(sb(st, "s_R", (128, 512), BF16))
                mo = Ring([sb(st, "s_mo%d" % i, (64, 512), BF16) for i in range(2)])
                psz = Ring([ps(st, "s_psz%d" % i) for i in range(2)])
                psl = Ring([ps(st, "s_psl%d" % i) for i in range(2)])
                pso = T(ps(st, "s_pso"))
                items = []
                for h in range(5):
                    for G in range(NG):
                        for kb in range(4 * (G + 1) - 1, -1, -1):
                            items.append((h, G, kb))
                hstate = {}

                def sb_front(h, G, kb):
                    nkb = 4 * (G + 1)
                    if G == 0 and kb == nkb - 1:
                        kt, qt, nq, v = ktr.next(), qtr.next(), nqr.next(), vr.next()
                        em.dma("sp", kt.t[:], projT[O_SK + 64 * h:O_SK + 64 * h + 64, :], writes=[kt])
                        em.dma("sp", qt.t[:], projT[O_SQ + 64 * h:O_SQ + 64 * h + 64, :], writes=[qt])
                        em.dma("sp", v.t[:], sbV[h], writes=[v])
                        em.op("dve", lambda e, nq=nq, qt=qt: e.tensor_scalar(nq.t[:], qt.t[:], -1.0, None, ALU.mult),
                              reads=[qt], writes=[nq])
                        hstate[h] = (kt, qt, nq, v)
                    kt, qt, nq, v = hstate[h]
                    t0 = G * 512
                    diag = kb >= 4 * G
                    pz = psz.next()
                    em.op("pe", lambda e, pz=pz, kt=kt, qt=qt, kb=kb, t0=t0: e.matmul(
                        pz.t[:], kt.t[:, kb * 128:(kb + 1) * 128], qt.t[:, t0:t0 + 512], start=True, stop=True),
                        reads=[kt, qt], writes=[pz])
                    E = er.next()
                    em.op("act", lambda e, pz=pz, E=E: e.activation(out=E.t[:], in_=pz.t[:], func=AF.Exp),
                          reads=[pz], writes=[E])
                    SP = spr.next()
                    em.op("act", lambda e, SP=SP, E=E: e.activation(out=SP.t[:], in_=E.t[:], func=AF.Ln, bias=1.0),
                          reads=[E], writes=[SP])
                    if diag:
                        em.op("pool", lambda e, SP=SP, kb=kb, G=G: e.tensor_tensor(
                            SP.t[:], SP.t[:], mstr.t[:, kb - 4 * G, :], ALU.mult), reads=[SP, mstr], writes=[SP])
                    return SP

                def sb_back(h, G, kb, SP):
                    kt, qt, nq, v = hstate[h]
                    t0 = G * 512
                    nkb = 4 * (G + 1)
                    first = (kb == nkb - 1)
                    diag = kb >= 4 * G
                    pl = psl.next()
                    em.op("pe", lambda e, pl=pl, SP=SP: e.matmul(pl.t[:], tri.t[:], SP.t[:], start=True, stop=False),
                          reads=[tri, SP], writes=[pl])
                    if not first:
                        em.op("pe", lambda e, pl=pl: e.matmul(pl.t[:], ones.t[:], R.t[:], start=False, stop=False),
                              reads=[ones, R], writes=[pl])
                    em.op("pe", lambda e, pl=pl, kt=kt, nq=nq, kb=kb, t0=t0: e.matmul(
                        pl.t[:], kt.t[:, kb * 128:(kb + 1) * 128], nq.t[:, t0:t0 + 512], start=False, stop=True),
                        reads=[kt, nq], writes=[pl])
                    Wt = wr.next()
                    em.op("act", lambda e, pl=pl, Wt=Wt: e.activation(out=Wt.t[:], in_=pl.t[:], func=AF.Exp, scale=-1.0),
                          reads=[pl], writes=[Wt])
                    if diag:
                        em.op("pool", lambda e, Wt=Wt, kb=kb, G=G: e.tensor_tensor(
                            Wt.t[:], Wt.t[:], mstr.t[:, kb - 4 * G, :], ALU.mult), reads=[Wt, mstr], writes=[Wt])
                    em.op("pe", lambda e, Wt=Wt, v=v, kb=kb, first=first: e.matmul(
                        pso.t[0:64, :], v.t[:, kb, :], Wt.t[:], start=first, stop=(kb == 0)),
                        reads=[v, Wt], writes=[pso])
                    if kb > 0:
                        if first:
                            em.op("pool", lambda e, SP=SP: e.tensor_copy(R.t[:], SP.t[:]), reads=[SP], writes=[R])
                        else:
                            em.op("pool", lambda e, SP=SP: e.tensor_tensor(R.t[:], R.t[:], SP.t[:], ALU.add),
                                  reads=[SP, R], writes=[R])
                    else:
                        m = mo.next()
                        em.op("dve", lambda e, m=m: e.tensor_copy(m.t[:], pso.t[0:64, :]), reads=[pso], writes=[m])
                        em.dma("sp", mixT[M_SB + 64 * h:M_SB + 64 * h + 64, t0:t0 + 512], m.t[:], reads=[m])

                cur = sb_front(*items[0])
                for i, it in enumerate(items):
                    nxt = sb_front(*items[i + 1]) if i + 1 < len(items) else None
                    sb_back(*it, cur)
                    cur = nxt
                em.flush()

            if upto < 4:
                return nc, em.nins
            with ExitStack() as st:
                ikT = T(sb(st, "d_ikT", (64, S), BF16))
                dkT = T(sb(st, "d_dkT", (64, S), BF16))
                dv = T(sb(st, "d_dv", (128, NBLK, 66), BF16))
                isc = T(sb(st, "d_isc", (128, S), F32))
                msk = T(sb(st, "d_msk", (128, S), BF16))
                mT = T(sb(st, "d_mT", (128, NBLK, 256), BF16))
                iqr = Ring([sb(st, "d_iq%d" % i, (64, 8, 128), BF16) for i in range(2)])
                iwr = Ring([sb(st, "d_iw%d" % i, (128, 8), F32) for i in range(2)])
                dqr = Ring([sb(st, "d_dq%d" % i, (64, 5, 256), BF16) for i in range(2)])
                rr = Ring([sb(st, "d_r%d" % i, (128, 512), BF16) for i in range(3)])
                ptr_ = Ring([sb(st, "d_pt%d" % i, (128, 256), BF16) for i in range(3)])
                sm = T(sb(st, "d_sm", (128, 16), F32))
                wi = T(sb(st, "d_wi", (128, NIT), F32))
                osb = T(sb(st, "d_osb", (128, 256), F32))
                rb_ = T(sb(st, "d_rb", (64, 256), F32))
                mo = Ring([sb(st, "d_mo%d" % i, (64, 256), BF16) for i in range(2)])
                psi = Ring([ps(st, "d_psi%d" % i) for i in range(3)])
                pst = T(ps(st, "d_pst", (128, 512), BF16))
                pss_ = Ring([ps(st, "d_pss%d" % i, (128, 256)) for i in range(2)])
                pso = T(ps(st, "d_pso", (128, 256)))
                psb_ = T(ps(st, "d_psb", (128, 256)))
                em.dma("sp", ikT.t[:], projT[O_IK:O_IK + 64, :], writes=[ikT])
                em.dma("sp", dkT.t[:], projT[O_DK:O_DK + 64, :], writes=[dkT])
                em.dma("sp", dv.t[:], dsaV, writes=[dv])
                LO, MID, CNT, STEP, MX, MN, THR = 0, 1, 2, 3, 4, 5, 6
                for G2 in range(S // 256):
                    q0 = G2 * 256
                    dq = dqr.next()
                    em.dma("sp", dq.t[:], projT[O_DQ:O_DQ + 320, q0:q0 + 256].rearrange("(h p) t -> p h t", p=64), writes=[dq])
                    for qs in range(2):
                        QB = 2 * G2 + qs
                        qb0 = QB * 128
                        Lk = (QB + 1) * 128
                        iq, iw = iqr.next(), iwr.next()
                        em.dma("sp", iq.t[:], projT[O_IQ:O_IQ + 512, qb0:qb0 + 128].rearrange("(h p) t -> p h t", p=64), writes=[iq])
                        em.dma("sp", iw.t[:], idxW[QB], writes=[iw])
                        nch = (Lk + 511) // 512
                        for c_ in range(nch):
                            k0 = c_ * 512
                            wc = min(512, Lk - k0)
                            for h in range(8):
                                p = psi.next()
                                em.op("pe", lambda e, p=p, iq=iq, h=h, k0=k0, wc=wc: e.matmul(
                                    p.t[:, 0:wc], iq.t[:, h, :], ikT.t[:, k0:k0 + wc], start=True, stop=True),
                                    reads=[iq, ikT], writes=[p])
                                r = rr.next()
                                em.op("act", lambda e, p=p, r=r, wc=wc: e.activation(out=r.t[:, 0:wc], in_=p.t[:, 0:wc], func=AF.Relu),
                                      reads=[p], writes=[r])
                                if h == 0:
                                    em.op("dve", lambda e, r=r, iw=iw, k0=k0, wc=wc: e.tensor_scalar(
                                        isc.t[:, k0:k0 + wc], r.t[:, 0:wc], iw.t[:, 0:1], None, ALU.mult),
                                        reads=[r, iw], writes=[isc])
                                else:
                                    em.op("dve", lambda e, r=r, iw=iw, h=h, k0=k0, wc=wc: e.scalar_tensor_tensor(
                                        isc.t[:, k0:k0 + wc], r.t[:, 0:wc], iw.t[:, h:h + 1], isc.t[:, k0:k0 + wc], ALU.mult, ALU.add),
                                        reads=[r, iw, isc], writes=[isc])
                        em.op("dve", lambda e, qb0=qb0: e.tensor_tensor(
                            isc.t[:, qb0:qb0 + 128], isc.t[:, qb0:qb0 + 128], cadd.t[:], ALU.add), reads=[isc, cadd], writes=[isc])
                        if Lk <= TOPK:
                            em.op("dve", lambda e: e.memset(sm.t[:, THR:THR + 1], -1.0e29), writes=[sm])
                        else:
                            em.op("dve", lambda e, Lk=Lk: e.tensor_reduce(sm.t[:, MX:MX + 1], isc.t[:, 0:Lk], AX.X, ALU.max),
                                  reads=[isc], writes=[sm])
                            em.op("dve", lambda e, Lk=Lk: e.tensor_reduce(sm.t[:, LO:LO + 1], isc.t[:, 0:Lk - 128], AX.X, ALU.min),
                                  reads=[isc], writes=[sm])
                            em.op("dve", lambda e: e.tensor_tensor(sm.t[:, MN:MN + 1], sm.t[:, MX:MX + 1], sm.t[:, LO:LO + 1], ALU.subtract),
                                  reads=[sm], writes=[sm])
                            em.op("dve", lambda e: e.tensor_scalar(wi.t[:], pow2.t[:], sm.t[:, MN:MN + 1], None, ALU.mult),
                                  reads=[sm, pow2], writes=[wi])
                            for it in range(NIT):
                                em.op("dve", lambda e, it=it: e.tensor_tensor(
                                    sm.t[:, MID:MID + 1], sm.t[:, LO:LO + 1], wi.t[:, it:it + 1], ALU.add), reads=[sm, wi], writes=[sm])
                                em.op("dve", lambda e, Lk=Lk: e.tensor_scalar(
                                    msk.t[:, 0:Lk], isc.t[:, 0:Lk], sm.t[:, MID:MID + 1], 0.0, ALU.is_ge, ALU.add,
                                    accum_out=sm.t[:, CNT:CNT + 1]), reads=[isc, sm], writes=[msk, sm])
                                em.op("dve", lambda e, it=it: e.tensor_scalar(
                                    sm.t[:, STEP:STEP + 1], sm.t[:, CNT:CNT + 1], TOPK - 0.5, wi.t[:, it:it + 1], ALU.is_ge, ALU.mult),
                                    reads=[sm, wi], writes=[sm])
                                em.op("dve", lambda e: e.tensor_tensor(
                                    sm.t[:, LO:LO + 1], sm.t[:, LO:LO + 1], sm.t[:, STEP:STEP + 1], ALU.add), reads=[sm], writes=[sm])
                            em.op("dve", lambda e: e.tensor_copy(sm.t[:, THR:THR + 1], sm.t[:, LO:LO + 1]), reads=[sm], writes=[sm])
                        em.op("dve", lambda e, Lk=Lk: e.tensor_scalar(
                            msk.t[:, 0:Lk], isc.t[:, 0:Lk], sm.t[:, THR:THR + 1], None, ALU.is_ge), reads=[isc, sm], writes=[msk])
                        nb = QB + 1
                        for k4 in range(0, nb, 4):
                            n4 = min(4, nb - k4)
                            for i in range(n4):
                                em.op("pe", lambda e, k4=k4, i=i: e.transpose(
                                    pst.t[:, i * 128:(i + 1) * 128], msk.t[:, (k4 + i) * 128:(k4 + i + 1) * 128], ident.t[:]),
                                    reads=[msk, ident], writes=[pst])
                            em.op("act", lambda e, k4=k4, n4=n4, qs=qs: e.copy(
                                mT.t[:, k4:k4 + n4, qs * 128:(qs + 1) * 128],
                                pst.t[:, 0:n4 * 128].rearrange("p (a q) -> p a q", q=128)), reads=[pst], writes=[mT])
                        if qs == 0:
                            em.op("pool", lambda e, QB=QB: e.memset(mT.t[:, QB + 1, 0:128], 0.0), writes=[mT])
                    nkb = 2 * G2 + 2
                    for h in range(5):
                        for kb in range(nkb):
                            p = pss_.next()
                            em.op("pe", lambda e, p=p, dq=dq, h=h, kb=kb: e.matmul(
                                p.t[:, :], dkT.t[:, kb * 128:(kb + 1) * 128], dq.t[:, h, :], start=True, stop=True),
                                reads=[dkT, dq], writes=[p])
                            pt = ptr_.next()
                            em.op("act", lambda e, p=p, pt=pt, h=h: e.activation(
                                out=pt.t[:], in_=p.t[:], func=AF.Exp, bias=b31.t[:, h:h + 1]), reads=[p, b31], writes=[pt])
                            em.op("pool", lambda e, pt=pt, kb=kb: e.tensor_tensor(pt.t[:], pt.t[:], mT.t[:, kb, :], ALU.mult),
                                  reads=[pt, mT], writes=[pt])
                            for qs in range(2):
                                dl = 2 * G2 + qs - kb
                                if dl in (0, 1):
                                    em.op("pool", lambda e, pt=pt, qs=qs, dl=dl, h=h: e.tensor_tensor(
                                        pt.t[:, qs * 128:(qs + 1) * 128], pt.t[:, qs * 128:(qs + 1) * 128], ctab.t[:, dl, h, :], ALU.mult),
                                        reads=[pt, ctab], writes=[pt])
                            em.op("pe", lambda e, pt=pt, kb=kb, nkb=nkb: e.matmul(
                                pso.t[0:65, :], dv.t[:, kb, 0:65], pt.t[:], start=(kb == 0), stop=(kb == nkb - 1)),
                                reads=[dv, pt], writes=[pso])
                        em.op("act", lambda e: e.copy(osb.t[0:65, :], pso.t[0:65, :]), reads=[pso], writes=[osb])
                        em.op("pe", lambda e: e.matmul(psb_.t[0:64, :], sel.t[0:65, :], osb.t[0:65, :], start=True, stop=True),
                              reads=[sel, osb], writes=[psb_])
                        em.op("dve", lambda e: e.reciprocal(rb_.t[:], psb_.t[0:64, :]), reads=[psb_], writes=[rb_])
                        m = mo.next()
                        em.op("dve", lambda e, m=m: e.tensor_tensor(m.t[:], osb.t[0:64, :], rb_.t[:], ALU.mult),
                              reads=[osb, rb_], writes=[m])
                        em.dma("sp", mixT[M_DSA + 64 * h:M_DSA + 64 * h + 64, q0:q0 + 256], m.t[:], reads=[m])
                em.flush()

            if upto < 5:
                return nc, em.nins
            with ExitStack() as st:
                wo = T(sb(st, "c_wo", (128, 8, D), BF16))
                mixr = Ring([sb(st, "c_mix%d" % i, (128, 8, 512), BF16) for i in range(2)])
                hinr = Ring([sb(st, "c_hin%d" % i, (128, 8, 512), F32) for i in range(2)])
                houtr = Ring([sb(st, "c_hout%d" % i, (128, 8, 512), F32) for i in range(2)])
                psr = Ring([ps(st, "c_ps%d" % i) for i in range(4)])
                wst = Ring([sb(st, "c_wst%d" % i, (128, WST), F32) for i in range(2)])
                wload(wo, W["w_o"][L], 8, wst)
                em.flush()
                for g in range(NG):
                    t0 = g * 512
                    mix, hin, hout = mixr.next(), hinr.next(), houtr.next()
                    em.dma("sp", mix.t[:], hview(mixT, t0), writes=[mix])
                    em.dma("sp", hin.t[:], hview(hsrc, t0), writes=[hin])
                    for fc in range(8):
                        p = psr.next()
                        for c_ in range(8):
                            em.op("pe", lambda e, p=p, c_=c_, fc=fc, mix=mix: e.matmul(
                                p.t[:], wo.t[:, c_, fc * 128:(fc + 1) * 128], mix.t[:, c_, :], start=(c_ == 0), stop=(c_ == 7)),
                                reads=[wo, mix], writes=[p])
                        em.op("dve", lambda e, p=p, fc=fc, hin=hin, hout=hout: e.tensor_tensor(
                            hout.t[:, fc, :], p.t[:], hin.t[:, fc, :], ALU.add), reads=[p, hin], writes=[hout])
                    em.dma("sp", hview(hT, t0), hout.t[:], reads=[hout])
                em.flush()

            if upto < 6:
                return nc, em.nins
            with ExitStack() as st:
                wg = T(sb(st, "f_wg", (128, 8, D_FF), BF16))
                wu = T(sb(st, "f_wu", (128, 8, D_FF), BF16))
                gf = T(sb(st, "f_gf", (128, 8), F32))
                hinr = Ring([sb(st, "f_hin%d" % i, (128, 8, 512), F32) for i in range(1)])
                sqt = T(sb(st, "f_sq", (128, 8, 512), BF16))
                hf = T(sb(st, "f_hf", (128, 8, 512), BF16))
                rs = T(sb(st, "f_rs", (128, 512), F32))
                sgr = Ring([sb(st, "f_sg%d" % i, (128, 512), F32) for i in range(2)])
                actr = Ring([sb(st, "f_act%d" % i, (128, NFF, 512), BF16) for i in range(1)])
                pss = T(ps(st, "f_pss"))
                psg = Ring([ps(st, "f_psg%d" % i) for i in range(3)])
                psu = Ring([ps(st, "f_psu%d" % i) for i in range(3)])
                wst = Ring([sb(st, "f_wst%d" % i, (128, WST), F32) for i in range(2)])
                wload(wg, W["w_gate"][L], 8, wst)
                wload(wu, W["w_up"][L], 8, wst)
                em.dma("sp", gf.t[:], W["g_ffn"][L], writes=[gf])
                em.flush()
                for g in range(NG):
                    t0 = g * 512
                    hin, at = hinr.next(), actr.next()
                    em.dma("sp", hin.t[:], hview(hT, t0), writes=[hin])
                    fm_norm(hin, gf, 8, sqt, pss, rs, 1.0 / D, EPS, hn=hf)
                    for f in range(NFF):
                        pg, pu = psg.next(), psu.next()
                        for (p, w_) in [(pg, wg), (pu, wu)]:
                            for c_ in range(8):
                                em.op("pe", lambda e, p=p, w_=w_, c_=c_, f=f: e.matmul(
                                    p.t[:], w_.t[:, c_, f * 128:(f + 1) * 128], hf.t[:, c_, :], start=(c_ == 0), stop=(c_ == 7)),
                                    reads=[w_, hf], writes=[p])
                        sg = sgr.next()
                        em.op("act", lambda e, pg=pg, sg=sg: e.activation(out=sg.t[:], in_=pg.t[:], func=AF.Silu), reads=[pg], writes=[sg])
                        em.op("dve", lambda e, pu=pu, sg=sg, at=at, f=f: e.tensor_tensor(at.t[:, f, :], sg.t[:], pu.t[:], ALU.mult),
                              reads=[pu, sg], writes=[at])
                    em.dma("sp", hview(actT, t0), at.t[:], reads=[at])
                em.flush()

            if upto < 7:
                return nc, em.nins
            with ExitStack() as st:
                last = (L == depth - 1)
                wd = T(sb(st, "g_wd", (128, NFF, D), BF16))
                wpg = T(sb(st, "g_wpg", (128, 8, D), BF16))
                wpp = T(sb(st, "g_wpp", (128, 2, D), BF16))
                gp = T(sb(st, "g_gp", (128, 8), F32))
                gfin = T(sb(st, "g_gfin", (128, 8), F32))
                atr = Ring([sb(st, "g_at%d" % i, (128, NFF, 512), BF16) for i in range(1)])
                hin = T(sb(st, "g_hin", (128, 8, 512), F32))
                h2 = T(sb(st, "g_h2", (128, 8, 512), F32))
                h3 = T(sb(st, "g_h3", (128, 8, 512), F32))
                sqt = T(sb(st, "g_sq", (128, 8, 512), BF16))
                hp = T(sb(st, "g_hp", (128, 8, 512), BF16))
                rs = T(sb(st, "g_rs", (128, 512), F32))
                ptl = Ring([sb(st, "g_pt%d" % i, (128, 2, 512), BF16) for i in range(1)])
                sgr = Ring([sb(st, "g_sg%d" % i, (128, 512), F32) for i in range(2)])
                pss = T(ps(st, "g_pss"))
                psr = Ring([ps(st, "g_ps%d" % i) for i in range(3)])
                psg = Ring([ps(st, "g_psg%d" % i) for i in range(2)])
                psp = Ring([ps(st, "g_psp%d" % i) for i in range(2)])
                wst = Ring([sb(st, "g_wst%d" % i, (128, 1024), F32) for i in range(2)])
                wload(wd, W["w_down"][L], NFF, wst)
                wload(wpg, W["w_ple_gate"][L], 8, wst)
                wload(wpp, W["w_ple_proj"][L], 2, wst)
                pstg = Ring([sb(st, "g_pstg%d" % i, (128, 2, 512), F32) for i in range(1)])
                em.dma("sp", gp.t[:], W["g_ple"][L], writes=[gp])
                em.dma("sp", gfin.t[:], W["g_fin"], writes=[gfin])
                em.flush()
                for g in range(NG):
                    t0 = g * 512
                    at, pt = atr.next(), ptl.next()
                    em.dma("sp", at.t[:], hview(actT, t0), writes=[at])
                    em.dma("sp", hin.t[:], hview(hT, t0), writes=[hin])
                    pq = pstg.next()
                    em.dma("sp", pq.t[:], pT[L].rearrange("(c p) t -> p c t", p=128)[:, :, t0:t0 + 512], writes=[pq])
                    em.op("pool", lambda e, pq=pq, pt=pt: e.tensor_copy(pt.t[:], pq.t[:]), reads=[pq], writes=[pt])
                    for fc in range(8):
                        p = psr.next()
                        for f in range(NFF):
                            em.op("pe", lambda e, p=p, f=f, fc=fc, at=at: e.matmul(
                                p.t[:], wd.t[:, f, fc * 128:(fc + 1) * 128], at.t[:, f, :], start=(f == 0), stop=(f == NFF - 1)),
                                reads=[wd, at], writes=[p])
                        em.op("dve", lambda e, p=p, fc=fc: e.tensor_tensor(h2.t[:, fc, :], p.t[:], hin.t[:, fc, :], ALU.add),
                              reads=[p, hin], writes=[h2])
                    fm_norm(h2, gp, 8, sqt, pss, rs, 1.0 / D, EPS, hn=hp)
                    for fc in range(8):
                        pg, pp_ = psg.next(), psp.next()
                        for c_ in range(8):
                            em.op("pe", lambda e, pg=pg, c_=c_, fc=fc: e.matmul(
                                pg.t[:], wpg.t[:, c_, fc * 128:(fc + 1) * 128], hp.t[:, c_, :], start=(c_ == 0), stop=(c_ == 7)),
                                reads=[wpg, hp], writes=[pg])
                        for j in range(2):
                            em.op("pe", lambda e, pp_=pp_, j=j, fc=fc, pt=pt: e.matmul(
                                pp_.t[:], wpp.t[:, j, fc * 128:(fc + 1) * 128], pt.t[:, j, :], start=(j == 0), stop=(j == 1)),
                                reads=[wpp, pt], writes=[pp_])
                        sg = sgr.next()
                        em.op("act", lambda e, pg=pg, sg=sg: e.activation(out=sg.t[:], in_=pg.t[:], func=AF.Sigmoid), reads=[pg], writes=[sg])
                        em.op("dve", lambda e, pp_=pp_, sg=sg: e.tensor_tensor(sg.t[:], sg.t[:], pp_.t[:], ALU.mult),
                              reads=[pp_, sg], writes=[sg])
                        em.op("pool", lambda e, sg=sg, fc=fc: e.tensor_tensor(h3.t[:, fc, :], sg.t[:], h2.t[:, fc, :], ALU.add),
                              reads=[sg, h2], writes=[h3])
                    if not last:
                        em.dma("sp", hview(hT, t0), h3.t[:], reads=[h3])
                    else:
                        fm_norm(h3, gfin, 8, sqt, pss, rs, 1.0 / D, EPS, hn=None)
                        for c_ in range(8):
                            em.op("dve", lambda e, c_=c_: e.scalar_tensor_tensor(
                                h2.t[:, c_, :], h3.t[:, c_, :], gfin.t[:, c_:c_ + 1], rs.t[:], ALU.mult, ALU.mult),
                                reads=[h3, gfin, rs], writes=[h2])
                        em.dma("sp", hview(yT, t0), h2.t[:], reads=[h2])
                em.flush()
    return nc, em.nins


def run(inp, S, depth, nb, debug=False, upto=99):
    w = prep_weights({k: np.asarray(v, np.float32) for k, v in inp.items() if k not in ("x", "p")})
    consts = make_consts(S)
    nc, nins = build(S, depth, debug, upto)
    x = np.asarray(inp["x"], np.float32)
    p = np.asarray(inp["p"], np.float32)
    in_maps = []
    for b in range(nb):
        m = {"xT": np.ascontiguousarray(x[b].T), "pT": np.ascontiguousarray(p[:, b].transpose(0, 2, 1))}
        m.update(w)
        for k, v in consts.items():
            m["c_" + k] = v
        in_maps.append(m)
    res = run_bass_kernel_spmd(nc, in_maps, core_ids=list(range(nb)))
    out = np.stack([np.ascontiguousarray(res.results[b]["yT"].T) for b in range(nb)], 0).astype(np.float32)
    return out, res


def kernel(**inputs):
    out, _ = run(inputs, SEQ, DEPTH, BATCH)
    return out
```

```python
import math
import os
import numpy as np
import ml_dtypes
import concourse.bass as bass
import concourse.mybir as mybir
from concourse.bass_utils import run_bass_kernel_spmd

F32 = mybir.dt.float32
BF16 = mybir.dt.bfloat16
AF = mybir.ActivationFunctionType
ALU = mybir.AluOpType
AX = mybir.AxisListType

D = 1024
DEPTH = 4
SEQ = 8192
BATCH = 4
D_IN = 2664
D_FF = 2816
NFF = D_FF // 128
EPS = 1e-6
NIT = 16
TOPK = 256
NEG = -1.0e30

O_CQ, O_CKV, O_KR, O_DQ, O_DK, O_DV, O_IQ, O_IK, O_IW, O_SQ, O_SK, O_SV = (
    0, 384, 640, 672, 992, 1056, 1120, 1632, 1696, 1704, 2024, 2344)
M_MLA, M_DSA, M_SB = 0, 384, 704

STRICT_SAME_ENGINE = os.environ.get("STRICT", "1") == "1"


class Ins:
    __slots__ = ("eng", "fn", "deps", "inc", "sem", "val", "epoch", "isdma", "slotwait")


class Buf:
    __slots__ = ("w", "r", "rd")

    def __init__(self):
        self.w = None
        self.r = {}
        self.rd = []


class T:
    def __init__(self, t):
        self.t = t
        self.b = Buf()


class Em:
    ENG = ("pe", "act", "dve", "pool", "sp")
    BLK = {"pe": "tensor", "act": "scalar", "dve": "vector", "pool": "gpsimd", "sp": "sync"}

    def __init__(self, nc, csems, dsems):
        self.nc = nc
        self.csem = csems
        self.dsem = dsems
        self.cnt = {e: 0 for e in csems}
        self.dcount = {q: 0 for q in dsems}
        self.dlast = {q: [None] * len(dsems[q]) for q in dsems}
        self.ops = {e: [] for e in self.ENG}
        self.seen = {e: {} for e in self.ENG}
        self.epoch = 0
        self.nins = 0

    def _mk(self, eng, fn, reads, writes, isdma):
        ins = Ins()
        ins.eng, ins.fn, ins.isdma, ins.inc, ins.epoch = eng, fn, isdma, False, self.epoch
        ins.sem = ins.val = ins.slotwait = None
        deps = set()
        for b in reads:
            if b.w is not None:
                deps.add(b.w)
        for b in writes:
            if b.w is not None:
                deps.add(b.w)
            deps.update(b.r.values())
            deps.update(b.rd)
        for b in reads:
            if isdma:
                b.rd.append(ins)
            else:
                b.r[eng] = ins
        for b in writes:
            b.w = ins
            b.r = {}
            b.rd = []
        out = []
        for d in deps:
            if d is ins or d.epoch != self.epoch:
                continue
            if (not isdma) and (not d.isdma) and d.eng == eng:
                if eng == "pe" or not STRICT_SAME_ENGINE:
                    continue
            out.append(d)
        ins.deps = out
        self.ops[eng].append(ins)
        self.nins += 1
        return ins

    def op(self, eng, fn, reads=(), writes=()):
        return self._mk(eng, fn, [x.b if isinstance(x, T) else x for x in reads],
                        [x.b if isinstance(x, T) else x for x in writes], False)

    def dma(self, q, out, in_, reads=(), writes=()):
        return self._mk(q, lambda e: e.dma_start(out=out, in_=in_),
                        [x.b if isinstance(x, T) else x for x in reads],
                        [x.b if isinstance(x, T) else x for x in writes], True)

    def flush(self):
        for e in self.ENG:
            for ins in self.ops[e]:
                for d in ins.deps:
                    d.inc = True
        for e in self.ENG:
            for ins in self.ops[e]:
                if ins.isdma:
                    n = len(self.dsem[e])
                    k = self.dcount[e]
                    self.dcount[e] += 1
                    slot = k % n
                    ins.sem = self.dsem[e][slot]
                    ins.val = 16 * (k // n + 1)
                    prev = self.dlast[e][slot]
                    if prev is not None and prev.epoch == self.epoch:
                        ins.slotwait = (prev.sem, prev.val)
                    self.dlast[e][slot] = ins
                elif ins.inc:
                    self.cnt[e] += 1
                    ins.sem = self.csem[e]
                    ins.val = self.cnt[e]
        finals = []
        for q in self.dsem:
            n = len(self.dsem[q])
            for slot in range(n):
                last = self.dlast[q][slot]
                if last is not None and last.epoch == self.epoch:
                    finals.append((last.sem, last.val))
        with self.nc.Block() as block:
            for e in self.ENG:
                ops = self.ops[e]
                seen = self.seen[e]

                def body(q, ops=ops, seen=seen, e=e):
                    for ins in ops:
                        waits = {}
                        for d in ins.deps:
                            key = id(d.sem)
                            if key not in waits or waits[key][1] < d.val:
                                waits[key] = (d.sem, d.val)
                        if ins.slotwait is not None:
                            key = id(ins.slotwait[0])
                            if key not in waits or waits[key][1] < ins.slotwait[1]:
                                waits[key] = ins.slotwait
                        for key, (sem, val) in waits.items():
                            if seen.get(key, 0) < val:
                                q.wait_ge(sem, val)
                                seen[key] = val
                        r = ins.fn(q)
                        if ins.isdma:
                            r.then_inc(ins.sem, 16)
                        elif ins.inc:
                            r.then_inc(ins.sem, 1)
                    if e == "sp":
                        for sem, val in finals:
                            if seen.get(id(sem), 0) < val:
                                q.wait_ge(sem, val)
                                seen[id(sem)] = val
                if ops or e == "sp":
                    getattr(block, self.BLK[e])(body)
        self.ops = {e: [] for e in self.ENG}
        self.epoch += 1


class Ring:
    def __init__(self, tiles):
        self.tiles = [T(t) for t in tiles]
        self.i = 0

    def next(self):
        t = self.tiles[self.i % len(self.tiles)]
        self.i += 1
        return t


def t5_bucket_np(dist):
    max_exact = 16
    d = np.maximum(dist, 1).astype(np.float32)
    large = max_exact + (np.log(d / np.float32(max_exact)) / np.float32(math.log(128 / max_exact))
                         * np.float32(32 - max_exact)).astype(np.int32)
    large = np.minimum(large, 31)
    return np.where(dist < max_exact, dist, large)


def make_consts(S):
    c = {}
    half = 16
    inv = (10000.0 ** (-np.arange(half, dtype=np.float32) / half)).astype(np.float32)
    ang = np.arange(S, dtype=np.float32)[:, None] * inv[None, :]
    cos = np.cos(ang).astype(np.float32).T
    sin = np.sin(ang).astype(np.float32).T
    c["cosT"] = np.ascontiguousarray(np.concatenate([cos, cos], 0))
    c["sinT"] = np.ascontiguousarray(np.concatenate([-sin, sin], 0))
    k = np.arange(128)[:, None]
    q = np.arange(512)[None, :]
    mi = np.zeros((4, 128, 512), np.float32)
    ms = np.zeros((4, 128, 512), np.float32)
    for kb in range(4):
        mi[kb] = ((kb * 128 + k) <= q)
        ms[kb] = ((kb * 128 + k) < q)
    c["mask_incl"] = mi.astype(ml_dtypes.bfloat16)
    c["mask_strict"] = ms.astype(ml_dtypes.bfloat16)
    j = np.arange(128)
    c["tri"] = (j[:, None] >= j[None, :]).astype(np.float32).astype(ml_dtypes.bfloat16)
    c["ones"] = np.ones((128, 128), ml_dtypes.bfloat16)
    c["ident"] = np.eye(128, dtype=np.float32).astype(ml_dtypes.bfloat16)
    sel = np.zeros((128, 64), np.float32)
    sel[64, :] = 1.0
    c["sel"] = sel
    oh = np.zeros((32, 128, 2, 128), np.float32)
    kk = np.arange(128)[:, None]
    qq = np.arange(128)[None, :]
    for dl in range(2):
        dist = np.maximum(128 * dl + qq - kk, 0)
        bk = t5_bucket_np(dist)
        for b in range(32):
            oh[b, :, dl, :] = (bk == b)
    c["oh"] = oh
    qi = np.arange(128)[:, None]
    ki = np.arange(128)[None, :]
    c["causal_add"] = np.where(ki <= qi, 0.0, NEG).astype(np.float32)
    c["pow2"] = np.tile((0.5 ** np.arange(1, NIT + 1)).astype(np.float32)[None, :], (128, 1))
    return c


CONST_SPECS = [("cosT", None, F32), ("sinT", None, F32), ("mask_incl", (4, 128, 512), BF16),
               ("mask_strict", (4, 128, 512), BF16), ("tri", (128, 128), BF16), ("ones", (128, 128), BF16),
               ("ident", (128, 128), BF16), ("sel", (128, 64), F32), ("oh", (32, 128, 2, 128), F32),
               ("causal_add", (128, 128), F32), ("pow2", (128, NIT), F32)]


def prep_weights(inp):
    w = {}
    w_in = inp["w_in"]
    L = w_in.shape[0]
    w["w_in"] = np.ascontiguousarray(w_in)
    w["w_tok"] = np.ascontiguousarray(np.concatenate(
        [w_in[:, :, O_DV:O_DV + 64], w_in[:, :, O_SV:O_SV + 320], w_in[:, :, O_IW:O_IW + 8]], axis=2))
    wkr = np.zeros((L, D, 2, 96), np.float32)
    wkr[:, :, 0, 64:96] = w_in[:, :, O_KR:O_KR + 32]
    wkr[:, :, 1, 64:80] = w_in[:, :, O_KR + 16:O_KR + 32]
    wkr[:, :, 1, 80:96] = w_in[:, :, O_KR:O_KR + 16]
    w["w_kr"] = wkr
    wuq = inp["mla_w_uq"]
    w["w_uq"] = np.ascontiguousarray(wuq)
    sw = wuq.reshape(L, 384, 6, 96).copy()
    sw[:, :, :, 64:80] = wuq.reshape(L, 384, 6, 96)[:, :, :, 80:96]
    sw[:, :, :, 80:96] = wuq.reshape(L, 384, 6, 96)[:, :, :, 64:80]
    w["w_uqs"] = np.ascontiguousarray(sw.reshape(L, 384, 576))
    wukv = inp["mla_w_ukv"].reshape(L, 256, 6, 128)
    w["w_uk"] = np.ascontiguousarray(wukv[:, :, :, :64].reshape(L, 256, 384))
    w["w_uv"] = np.ascontiguousarray(wukv[:, :, :, 64:].reshape(L, 256, 384))
    g = lambda a, n: np.ascontiguousarray(a.reshape(L, n, 128).transpose(0, 2, 1))
    w["g_attn"] = g(inp["attn_norm"], 8)
    w["g_ffn"] = g(inp["ffn_norm"], 8)
    w["g_ple"] = g(inp["ple_norm"], 8)
    w["g_q"] = g(inp["mla_q_norm"], 3)
    w["g_kv"] = g(inp["mla_kv_norm"], 2)
    w["g_fin"] = np.ascontiguousarray(inp["final_norm"].reshape(8, 128).T)
    for k in ("w_o", "w_gate", "w_up", "w_down", "w_ple_gate", "w_ple_proj", "rel_bias"):
        w[k] = np.ascontiguousarray(inp[k])
    return w


def build(S, depth, debug=False, upto=99):
    NG = S // 512
    NBLK = S // 128
    nc = bass.Bass("TRN2", target_bir_lowering=False)
    dt_in = lambda name, shape, dt=F32: nc.dram_tensor(name, list(shape), dt, kind="ExternalInput").ap()
    dkind = "ExternalOutput" if debug else "Internal"
    dt_sc = lambda name, shape, dt: nc.dram_tensor(name, list(shape), dt, kind=dkind).ap()

    xT = dt_in("xT", (D, S))
    pT = dt_in("pT", (depth, 256, S))
    W = {}
    for name, shape in [("w_in", (depth, D, D_IN)), ("w_tok", (depth, D, 392)), ("w_kr", (depth, D, 2, 96)),
                        ("w_uq", (depth, 384, 576)), ("w_uqs", (depth, 384, 576)), ("w_uk", (depth, 256, 384)),
                        ("w_uv", (depth, 256, 384)), ("g_attn", (depth, 128, 8)), ("g_ffn", (depth, 128, 8)),
                        ("g_ple", (depth, 128, 8)), ("g_q", (depth, 128, 3)), ("g_kv", (depth, 128, 2)),
                        ("g_fin", (128, 8)), ("w_o", (depth, D, D)), ("w_gate", (depth, D, D_FF)),
                        ("w_up", (depth, D, D_FF)), ("w_down", (depth, D_FF, D)), ("w_ple_gate", (depth, D, D)),
                        ("w_ple_proj", (depth, 256, D)), ("rel_bias", (32, 5))]:
        W[name] = dt_in(name, shape)
    C = {}
    for name, shape, dt in CONST_SPECS:
        if shape is None:
            shape = (32, S)
        C[name] = dt_in("c_" + name, shape, dt)
    yT = nc.dram_tensor("yT", [D, S], F32, kind="ExternalOutput").ap()

    hT = dt_sc("hT", (D, S), F32)
    projT = dt_sc("projT", (D_IN, S), BF16)
    mlaQT = dt_sc("mlaQT", (6, 96, S), BF16)
    mlaKT = dt_sc("mlaKT", (6, 96, S), BF16)
    mlaV = dt_sc("mlaV", (6, 128, NBLK, 66), BF16)
    dsaV = dt_sc("dsaV", (128, NBLK, 66), BF16)
    sbV = dt_sc("sbV", (5, 128, NBLK, 64), BF16)
    idxW = dt_sc("idxW", (NBLK, 128, 8), F32)
    mixT = dt_sc("mixT", (D, S), BF16)
    actT = dt_sc("actT", (D_FF, S), BF16)

    from contextlib import ExitStack
    top = ExitStack()
    with top:
        NSP, NPQ = 16, 8
        csems = {e: top.enter_context(nc.semaphore("s_" + e)) for e in ("pe", "act", "dve", "pool")}
        dsems = {"sp": [top.enter_context(nc.semaphore("d_sp%d" % i)) for i in range(NSP)]}
        em = Em(nc, csems, dsems)

        uid = [0]

        def sb(stack, name, shape, dt):
            uid[0] += 1
            return stack.enter_context(nc.sbuf_tensor("%s_%d" % (name, uid[0]), list(shape), dt))

        def ps(stack, name, shape=(128, 512), dt=F32):
            uid[0] += 1
            return stack.enter_context(nc.psum_tensor("%s_%d" % (name, uid[0]), list(shape), dt))

        ones = T(sb(top, "k_ones", (128, 128), BF16))
        tri = T(sb(top, "k_tri", (128, 128), BF16))
        ident = T(sb(top, "k_ident", (128, 128), BF16))
        sel = T(sb(top, "k_sel", (128, 64), F32))
        mincl = T(sb(top, "k_mincl", (128, 4, 512), BF16))
        mstr = T(sb(top, "k_mstr", (128, 4, 512), BF16))
        cadd = T(sb(top, "k_cadd", (128, 128), F32))
        pow2 = T(sb(top, "k_pow2", (128, NIT), F32))
        ctab = T(sb(top, "k_ctab", (128, 2, 5, 128), BF16))
        b31 = T(sb(top, "k_b31", (128, 5), F32))

        with ExitStack() as st:
            oh = T(sb(st, "s_oh", (128, 32, 256), F32))
            rbb = T(sb(st, "s_rbb", (128, 160), F32))
            dcol = T(sb(st, "s_dcol", (128, 32, 5), F32))
            acc = T(sb(st, "s_acc", (128, 5, 256), F32))
            em.dma("sp", ones.t[:], C["ones"], writes=[ones])
            em.dma("sp", tri.t[:], C["tri"], writes=[tri])
            em.dma("sp", ident.t[:], C["ident"], writes=[ident])
            em.dma("sp", sel.t[:], C["sel"], writes=[sel])
            em.dma("sp", mincl.t[:], C["mask_incl"].rearrange("a p q -> p a q"), writes=[mincl])
            em.dma("sp", mstr.t[:], C["mask_strict"].rearrange("a p q -> p a q"), writes=[mstr])
            em.dma("sp", cadd.t[:], C["causal_add"], writes=[cadd])
            em.dma("sp", pow2.t[:], C["pow2"], writes=[pow2])
            for b in range(32):
                em.dma("sp", oh.t[:, b, :], C["oh"][b].rearrange("p a q -> p (a q)"), writes=[oh])
            rb_flat = W["rel_bias"].rearrange("b h -> (b h)")
            em.dma("sp", rbb.t[:], bass.AP(rb_flat.tensor, rb_flat.offset, [[0, 128], [1, 160]]), writes=[rbb])
            rb3 = rbb.t[:].rearrange("p (b h) -> p b h", h=5)
            em.op("dve", lambda e: e.tensor_copy(b31.t[:], rb3[:, 31, :]), reads=[rbb], writes=[b31])
            for h in range(5):
                em.op("dve", lambda e, h=h: e.tensor_scalar(dcol.t[:, :, h], rb3[:, :, h], b31.t[:, h:h + 1], None,
                                                            ALU.subtract), reads=[rbb, b31], writes=[dcol])
            for h in range(5):
                for b in range(31):
                    if b == 0:
                        em.op("dve", lambda e, h=h, b=b: e.tensor_scalar(
                            acc.t[:, h, :], oh.t[:, b, :], dcol.t[:, b, h:h + 1], None, ALU.mult),
                            reads=[oh, dcol], writes=[acc])
                    else:
                        em.op("dve", lambda e, h=h, b=b: e.scalar_tensor_tensor(
                            acc.t[:, h, :], oh.t[:, b, :], dcol.t[:, b, h:h + 1], acc.t[:, h, :], ALU.mult, ALU.add),
                            reads=[oh, dcol, acc], writes=[acc])
            for h in range(5):
                for dl in range(2):
                    em.op("act", lambda e, h=h, dl=dl: e.activation(
                        out=ctab.t[:, dl, h, :], in_=acc.t[:, h, dl * 128:(dl + 1) * 128], func=AF.Exp),
                        reads=[acc], writes=[ctab])
            em.flush()
        if upto < 1:
            return nc, em.nins

        WST = 1408

        def wload(dst, src, nchunk, wst):
            v = src.rearrange("(c p) n -> p c n", p=128)
            n = v.shape[2]
            for c_ in range(nchunk):
                for n0 in range(0, n, WST):
                    n1 = min(n, n0 + WST)
                    s_ = wst.next()
                    em.dma("sp", s_.t[:, 0:n1 - n0], v[:, c_, n0:n1], writes=[s_])
                    em.op("pool", lambda e, s_=s_, c_=c_, n0=n0, n1=n1: e.tensor_copy(dst.t[:, c_, n0:n1], s_.t[:, 0:n1 - n0]),
                          reads=[s_], writes=[dst])

        def fm_norm(hin, gain, nchunk, sqt, psb, rs, scale, eps, hn=None, out_f32=None):
            em.op("act", lambda e: e.activation(out=sqt.t[:, 0:nchunk, :], in_=hin.t[:, 0:nchunk, :], func=AF.Square),
                  reads=[hin], writes=[sqt])
            for c_ in range(nchunk):
                em.op("pe", lambda e, c_=c_: e.matmul(psb.t[:], ones.t[:], sqt.t[:, c_, :],
                                                      start=(c_ == 0), stop=(c_ == nchunk - 1)),
                      reads=[ones, sqt], writes=[psb])
            em.op("dve", lambda e: e.tensor_scalar(rs.t[:], psb.t[:], scale, eps, ALU.mult, ALU.add),
                  reads=[psb], writes=[rs])
            em.op("act", lambda e: e.activation(out=rs.t[:], in_=rs.t[:], func=AF.Sqrt), reads=[rs], writes=[rs])
            em.op("dve", lambda e: e.reciprocal(rs.t[:], rs.t[:]), reads=[rs], writes=[rs])
            if hn is not None:
                for c_ in range(nchunk):
                    em.op("dve", lambda e, c_=c_: e.scalar_tensor_tensor(
                        hn.t[:, c_, :], hin.t[:, c_, :], gain.t[:, c_:c_ + 1], rs.t[:], ALU.mult, ALU.mult),
                        reads=[hin, gain, rs], writes=[hn])

        hview = lambda ap, t0, n=512: ap.rearrange("(c p) t -> p c t", p=128)[:, :, t0:t0 + n]

        for L in range(depth):
            hsrc = xT if L == 0 else hT
            with ExitStack() as st:
                win = T(sb(st, "a_win", (128, 8, D_IN), BF16))
                wtok = T(sb(st, "a_wtok", (128, 8, 392), BF16))
                wkr = T(sb(st, "a_wkr", (128, 8, 192), BF16))
                wuq = T(sb(st, "a_wuq", (128, 3, 576), BF16))
                wuqs = T(sb(st, "a_wuqs", (128, 3, 576), BF16))
                wuk = T(sb(st, "a_wuk", (128, 2, 384), BF16))
                wuv = T(sb(st, "a_wuv", (128, 2, 384), BF16))
                gat = T(sb(st, "a_gat", (128, 8), F32))
                gq = T(sb(st, "a_gq", (128, 3), F32))
                gkv = T(sb(st, "a_gkv", (128, 2), F32))
                hin = T(sb(st, "a_hin", (128, 8, 512), F32))
                sqt = T(sb(st, "a_sq", (128, 8, 512), BF16))
                hn = T(sb(st, "a_hn", (128, 8, 512), BF16))
                rs = T(sb(st, "a_rs", (128, 512), F32))
                rq = T(sb(st, "a_rq", (128, 512), F32))
                rkv = T(sb(st, "a_rkv", (128, 512), F32))
                cs = T(sb(st, "a_cs", (128, 2, 512), F32))
                raw = T(sb(st, "a_raw", (128, 5, 512), F32))
                sq2 = T(sb(st, "a_sq2", (128, 5, 512), BF16))
                cqn = T(sb(st, "a_cqn", (128, 3, 512), BF16))
                ckvn = T(sb(st, "a_ckvn", (128, 2, 512), BF16))
                stg = Ring([sb(st, "a_stg%d" % i, (128, 512), BF16) for i in range(4)])
                t1r = Ring([sb(st, "a_t1%d" % i, (128, 512), F32) for i in range(2)])
                t2r = Ring([sb(st, "a_t2%d" % i, (128, 512), F32) for i in range(2)])
                dvt = T(sb(st, "a_dvt", (128, 4, 66), BF16))
                svt = T(sb(st, "a_svt", (128, 4, 320), BF16))
                iwt = T(sb(st, "a_iwt", (128, 4, 8), F32))
                mvt = T(sb(st, "a_mvt", (128, 4, 6, 66), BF16))
                psr = Ring([ps(st, "a_ps%d" % i) for i in range(6)])
                pss = T(ps(st, "a_pss"))
                pss2 = T(ps(st, "a_pss2"))

                ABITS0 = int(os.environ.get('A_BITS', '63'))
                if not (ABITS0 & 64):
                    wst = Ring([sb(st, "a_wst%d" % i, (128, WST), F32) for i in range(2)])
                    wload(win, W["w_in"][L], 8, wst)
                    wload(wtok, W["w_tok"][L], 8, wst)
                    wload(wkr, W["w_kr"][L].rearrange("d a m -> d (a m)"), 8, wst)
                    wload(wuq, W["w_uq"][L], 3, wst)
                    wload(wuqs, W["w_uqs"][L], 3, wst)
                    wload(wuk, W["w_uk"][L], 2, wst)
                    wload(wuv, W["w_uv"][L], 2, wst)
                em.dma("sp", gat.t[:], W["g_attn"][L], writes=[gat])
                em.dma("sp", gq.t[:], W["g_q"][L], writes=[gq])
                em.dma("sp", gkv.t[:], W["g_kv"][L], writes=[gkv])
                em.op("pool", lambda e: e.memset(dvt.t[:], 1.0), writes=[dvt])
                em.op("pool", lambda e: e.memset(mvt.t[:], 1.0), writes=[mvt])
                if os.environ.get("SPLITW", "1") == "1":
                    em.flush()
                evq = [0]

                def evac(dst_ap, src_ap, reads, writes, scale=None):
                    evq[0] += 1
                    if evq[0] % 2 == 0:
                        if scale is None:
                            em.op("act", lambda e: e.copy(dst_ap, src_ap), reads=reads, writes=writes)
                        else:
                            em.op("act", lambda e: e.mul(dst_ap, src_ap, scale), reads=reads, writes=writes)
                    else:
                        if scale is None:
                            em.op("dve", lambda e: e.tensor_copy(dst_ap, src_ap), reads=reads, writes=writes)
                        else:
                            em.op("dve", lambda e: e.tensor_scalar(dst_ap, src_ap, scale, None, ALU.mult),
                                  reads=reads, writes=writes)

                apasses = [int(x) for x in os.environ.get('A_PASSES', '63').split(',')]
                for (ABITS, g) in [(ab, g_) for ab in apasses for g_ in range(NG)]:
                    if g == 0 and ABITS != apasses[0]:
                        em.flush()
                    t0 = g * 512
                    em.dma("sp", hin.t[:], hview(hsrc, t0), writes=[hin])
                    em.dma("sp", cs.t[64:96, 0, :], C["cosT"][:, t0:t0 + 512], writes=[cs])
                    em.dma("sp", cs.t[64:96, 1, :], C["sinT"][:, t0:t0 + 512], writes=[cs])
                    fm_norm(hin, gat, 8, sqt, pss, rs, 1.0 / D, EPS, hn=hn)
                    for (c0, n, scale) in [] if not (ABITS & 1) else [(O_DQ, 320, 0.125), (O_DK, 64, None), (O_IQ, 512, None), (O_IK, 64, None),
                                           (O_SQ, 320, 0.125), (O_SK, 320, None)]:
                        m0 = 0
                        while m0 < n:
                            M = min(128, n - m0)
                            p = psr.next()
                            for c_ in range(8):
                                em.op("pe", lambda e, p=p, c_=c_, a=c0 + m0, M=M: e.matmul(
                                    p.t[0:M, :], win.t[:, c_, a:a + M], hn.t[:, c_, :], start=(c_ == 0), stop=(c_ == 7)),
                                    reads=[win, hn], writes=[p])
                            o = stg.next()
                            evac(o.t[0:M, :], p.t[0:M, :], [p], [o], scale)
                            em.dma("sp", projT[c0 + m0:c0 + m0 + M, t0:t0 + 512], o.t[0:M, :], reads=[o])
                            m0 += M
                    if not (ABITS & 2):
                        continue
                    for j in range(5):
                        p = psr.next()
                        for c_ in range(8):
                            em.op("pe", lambda e, p=p, c_=c_, j=j: e.matmul(
                                p.t[:], win.t[:, c_, j * 128:(j + 1) * 128], hn.t[:, c_, :], start=(c_ == 0), stop=(c_ == 7)),
                                reads=[win, hn], writes=[p])
                        evac(raw.t[:, j, :], p.t[:], [p], [raw])
                    em.op("act", lambda e: e.activation(out=sq2.t[:], in_=raw.t[:], func=AF.Square), reads=[raw], writes=[sq2])
                    for j in range(3):
                        em.op("pe", lambda e, j=j: e.matmul(pss.t[:], ones.t[:], sq2.t[:, j, :], start=(j == 0), stop=(j == 2)),
                              reads=[ones, sq2], writes=[pss])
                    for j in range(2):
                        em.op("pe", lambda e, j=j: e.matmul(pss2.t[:], ones.t[:], sq2.t[:, 3 + j, :], start=(j == 0), stop=(j == 1)),
                              reads=[ones, sq2], writes=[pss2])
                    for (r_, p_, sc_, ep_) in [(rq, pss, 96.0 / 384.0, 96.0 * EPS), (rkv, pss2, 1.0 / 256.0, EPS)]:
                        em.op("dve", lambda e, r_=r_, p_=p_, sc_=sc_, ep_=ep_: e.tensor_scalar(
                            r_.t[:], p_.t[:], sc_, ep_, ALU.mult, ALU.add), reads=[p_], writes=[r_])
                        em.op("act", lambda e, r_=r_: e.activation(out=r_.t[:], in_=r_.t[:], func=AF.Sqrt), reads=[r_], writes=[r_])
                        em.op("dve", lambda e, r_=r_: e.reciprocal(r_.t[:], r_.t[:]), reads=[r_], writes=[r_])
                    for j in range(3):
                        em.op("dve", lambda e, j=j: e.scalar_tensor_tensor(
                            cqn.t[:, j, :], raw.t[:, j, :], gq.t[:, j:j + 1], rq.t[:], ALU.mult, ALU.mult),
                            reads=[raw, gq, rq], writes=[cqn])
                    for j in range(2):
                        em.op("dve", lambda e, j=j: e.scalar_tensor_tensor(
                            ckvn.t[:, j, :], raw.t[:, 3 + j, :], gkv.t[:, j:j + 1], rkv.t[:], ALU.mult, ALU.mult),
                            reads=[raw, gkv, rkv], writes=[ckvn])

                    def rope_out(pa, pb, o):
                        a_, b_ = t1r.next(), t2r.next()
                        em.op("dve", lambda e: e.tensor_tensor(a_.t[64:96, :], pa.t[64:96, :], cs.t[64:96, 0, :], ALU.mult),
                              reads=[pa, cs], writes=[a_])
                        em.op("dve", lambda e: e.tensor_tensor(b_.t[64:96, :], pb.t[64:96, :], cs.t[64:96, 1, :], ALU.mult),
                              reads=[pb, cs], writes=[b_])
                        em.op("pool", lambda e: e.tensor_tensor(o.t[64:96, :], a_.t[64:96, :], b_.t[64:96, :], ALU.add),
                              reads=[a_, b_], writes=[o])

                    for h in range(6 if (ABITS & 4) else 0):
                        pa, pb = psr.next(), psr.next()
                        for (p, w_) in [(pa, wuq), (pb, wuqs)]:
                            for j in range(3):
                                em.op("pe", lambda e, p=p, w_=w_, j=j, h=h: e.matmul(
                                    p.t[0:96, :], w_.t[:, j, h * 96:(h + 1) * 96], cqn.t[:, j, :], start=(j == 0), stop=(j == 2)),
                                    reads=[w_, cqn], writes=[p])
                        o = stg.next()
                        em.op("act", lambda e, o=o, pa=pa: e.copy(o.t[0:64, :], pa.t[0:64, :]), reads=[pa], writes=[o])
                        rope_out(pa, pb, o)
                        em.dma("sp", mlaQT[h, :, t0:t0 + 512], o.t[0:96, :], reads=[o])
                    for h in range(6 if (ABITS & 8) else 0):
                        p = psr.next()
                        for j in range(2):
                            em.op("pe", lambda e, p=p, j=j, h=h: e.matmul(
                                p.t[0:64, :], wuk.t[:, j, h * 64:(h + 1) * 64], ckvn.t[:, j, :], start=(j == 0), stop=(j == 1)),
                                reads=[wuk, ckvn], writes=[p])
                        o = stg.next()
                        evac(o.t[0:64, :], p.t[0:64, :], [p], [o])
                        em.dma("sp", mlaKT[h, 0:64, t0:t0 + 512], o.t[0:64, :], reads=[o])
                    if ABITS & 16:
                        pa, pb = psr.next(), psr.next()
                        for (p, v_) in [(pa, 0), (pb, 1)]:
                            for c_ in range(8):
                                em.op("pe", lambda e, p=p, v_=v_, c_=c_: e.matmul(
                                    p.t[0:96, :], wkr.t[:, c_, v_ * 96:(v_ + 1) * 96], hn.t[:, c_, :], start=(c_ == 0), stop=(c_ == 7)),
                                    reads=[wkr, hn], writes=[p])
                        o = stg.next()
                        rope_out(pa, pb, o)
                        for h in range(6):
                            em.dma("sp", mlaKT[h, 64:96, t0:t0 + 512], o.t[64:96, :], reads=[o])
                    if not (ABITS & 32):
                        continue
                    for blk in range(4):
                        if not (ABITS & 4096):
                            p = psr.next()
                            for c_ in range(8):
                                em.op("pe", lambda e, p=p, c_=c_, blk=blk: e.matmul(
                                    p.t[:, 0:392], hn.t[:, c_, blk * 128:(blk + 1) * 128], wtok.t[:, c_, :], start=(c_ == 0), stop=(c_ == 7)),
                                    reads=[wtok, hn], writes=[p])
                            if not (ABITS & 16384):
                                em.op("dve", lambda e, p=p, blk=blk: e.tensor_copy(dvt.t[:, blk, 0:64], p.t[:, 0:64]), reads=[p], writes=[dvt])
                            em.op("dve", lambda e, p=p, blk=blk: e.tensor_copy(svt.t[:, blk, :], p.t[:, 64:384]), reads=[p], writes=[svt])
                            if not (ABITS & 32768):
                                em.op("dve", lambda e, p=p, blk=blk: e.tensor_copy(iwt.t[:, blk, :], p.t[:, 384:392]), reads=[p], writes=[iwt])
                        if not (ABITS & 8192):
                            p2 = psr.next()
                            for j in range(2):
                                em.op("pe", lambda e, p2=p2, j=j, blk=blk: e.matmul(
                                    p2.t[:, 0:384], ckvn.t[:, j, blk * 128:(blk + 1) * 128], wuv.t[:, j, :], start=(j == 0), stop=(j == 1)),
                                    reads=[wuv, ckvn], writes=[p2])
                            em.op("dve", lambda e, p2=p2, blk=blk: e.tensor_copy(
                                mvt.t[:, blk, :, 0:64], p2.t[:, 0:384].rearrange("p (h d) -> p h d", d=64)), reads=[p2], writes=[mvt])
                    if not (ABITS & 256):
                        em.dma("sp", dsaV[:, 4 * g:4 * g + 4, :], dvt.t[:], reads=[dvt])
                    if not (ABITS & 512):
                        for h in range(5):
                            em.dma("sp", sbV[h, :, 4 * g:4 * g + 4, :], svt.t[:, :, h * 64:(h + 1) * 64], reads=[svt])
                    if not (ABITS & 1024):
                        for h in range(6):
                            em.dma("sp", mlaV[h, :, 4 * g:4 * g + 4, :], mvt.t[:, :, h, :], reads=[mvt])
                    if not (ABITS & 2048):
                        em.dma("sp", idxW.rearrange("b p h -> p b h")[:, 4 * g:4 * g + 4, :], iwt.t[:], reads=[iwt])
                em.flush()

            if upto < 2:
                return nc, em.nins
            with ExitStack() as st:
                ktr = Ring([sb(st, "m_kt%d" % i, (96, S), BF16) for i in range(2)])
                qtr = Ring([sb(st, "m_qt%d" % i, (96, S), BF16) for i in range(2)])
                vr = Ring([sb(st, "m_v%d" % i, (128, NBLK, 66), BF16) for i in range(2)])
                ptr_ = Ring([sb(st, "m_pt%d" % i, (128, 512), BF16) for i in range(3)])
                osb = T(sb(st, "m_osb", (128, 512), F32))
                rb_ = T(sb(st, "m_rb", (64, 512), F32))
                mo = Ring([sb(st, "m_mo%d" % i, (64, 512), BF16) for i in range(2)])
                pss_ = Ring([ps(st, "m_ps%d" % i) for i in range(3)])
                pso = T(ps(st, "m_pso"))
                psb_ = T(ps(st, "m_psb"))
                items = []
                for h in range(6):
                    for G in range(NG):
                        for kb in range(4 * (G + 1)):
                            items.append((h, G, kb))
                hstate = {}

                def mla_front(h, G, kb):
                    if G == 0 and kb == 0:
                        kt, qt, v = ktr.next(), qtr.next(), vr.next()
                        em.dma("sp", kt.t[:], mlaKT[h], writes=[kt])
                        em.dma("sp", qt.t[:], mlaQT[h], writes=[qt])
                        em.dma("sp", v.t[:], mlaV[h], writes=[v])
                        hstate[h] = (kt, qt, v)
                    kt, qt, v = hstate[h]
                    t0 = G * 512
                    p = pss_.next()
                    em.op("pe", lambda e, p=p, kt=kt, qt=qt, kb=kb, t0=t0: e.matmul(
                        p.t[:], kt.t[0:96, kb * 128:(kb + 1) * 128], qt.t[0:96, t0:t0 + 512], start=True, stop=True),
                        reads=[kt, qt], writes=[p])
                    pt = ptr_.next()
                    em.op("act", lambda e, p=p, pt=pt: e.activation(out=pt.t[:], in_=p.t[:], func=AF.Exp),
                          reads=[p], writes=[pt])
                    if kb >= 4 * G:
                        em.op("pool", lambda e, pt=pt, kb=kb, G=G: e.tensor_tensor(
                            pt.t[:], pt.t[:], mincl.t[:, kb - 4 * G, :], ALU.mult), reads=[pt, mincl], writes=[pt])
                    return pt

                def mla_back(h, G, kb, pt):
                    kt, qt, v = hstate[h]
                    t0 = G * 512
                    nkb = 4 * (G + 1)
                    em.op("pe", lambda e, pt=pt, v=v, kb=kb, nkb=nkb: e.matmul(
                        pso.t[0:65, :], v.t[:, kb, 0:65], pt.t[:], start=(kb == 0), stop=(kb == nkb - 1)),
                        reads=[v, pt], writes=[pso])
                    if kb == nkb - 1:
                        em.op("act", lambda e: e.copy(osb.t[0:65, :], pso.t[0:65, :]), reads=[pso], writes=[osb])
                        em.op("pe", lambda e: e.matmul(psb_.t[0:64, :], sel.t[0:65, :], osb.t[0:65, :], start=True, stop=True),
                              reads=[sel, osb], writes=[psb_])
                        em.op("dve", lambda e: e.reciprocal(rb_.t[:], psb_.t[0:64, :]), reads=[psb_], writes=[rb_])
                        m = mo.next()
                        em.op("dve", lambda e, m=m: e.tensor_tensor(m.t[:], osb.t[0:64, :], rb_.t[:], ALU.mult),
                              reads=[osb, rb_], writes=[m])
                        em.dma("sp", mixT[M_MLA + 64 * h:M_MLA + 64 * h + 64, t0:t0 + 512], m.t[:], reads=[m])

                cur = mla_front(*items[0])
                for i, it in enumerate(items):
                    nxt = mla_front(*items[i + 1]) if i + 1 < len(items) else None
                    mla_back(*it, cur)
                    cur = nxt
                em.flush()

            if upto < 3:
                return nc, em.nins
            with ExitStack() as st:
                ktr = Ring([sb(st, "s_kt%d" % i, (64, S), BF16) for i in range(2)])
                qtr = Ring([sb(st, "s_qt%d" % i, (64, S), BF16) for i in range(2)])
                nqr = Ring([sb(st, "s_nq%d" % i, (64, S), BF16) for i in range(2)])
                vr = Ring([sb(st, "s_v%d" % i, (128, NBLK, 64), BF16) for i in range(2)])
                er = Ring([sb(st, "s_e%d" % i, (128, 512), F32) for i in range(2)])
                spr = Ring([sb(st, "s_sp%d" % i, (128, 512), BF16) for i in range(3)])
                wr = Ring([sb(st, "s_w%d" % i, (128, 512), BF16) for i in range(3)])
                R = T(sb(st, "s_R", (128, 512), BF16))
                mo = Ring([sb(st, "s_mo%d" % i, (64, 512), BF16) for i in range(2)])
                psz = Ring([ps(st, "s_psz%d" % i) for i in range(2)])
                psl = Ring([ps(st, "s_psl%d" % i) for i in range(2)])
                pso = T(ps(st, "s_pso"))
                items = []
                for h in range(5):
                    for G in range(NG):
                        for kb in range(4 * (G + 1) - 1, -1, -1):
                            items.append((h, G, kb))
                hstate = {}

                def sb_front(h, G, kb):
                    nkb = 4 * (G + 1)
                    if G == 0 and kb == nkb - 1:
                        kt, qt, nq, v = ktr.next(), qtr.next(), nqr.next(), vr.next()
                        em.dma("sp", kt.t[:], projT[O_SK + 64 * h:O_SK + 64 * h + 64, :], writes=[kt])
                        em.dma("sp", qt.t[:], projT[O_SQ + 64 * h:O_SQ + 64 * h + 64, :], writes=[qt])
                        em.dma("sp", v.t[:], sbV[h], writes=[v])
                        em.op("dve", lambda e, nq=nq, qt=qt: e.tensor_scalar(nq.t[:], qt.t[:], -1.0, None, ALU.mult),
                              reads=[qt], writes=[nq])
                        hstate[h] = (kt, qt, nq, v)
                    kt, qt, nq, v = hstate[h]
                    t0 = G * 512
                    diag = kb >= 4 * G
                    pz = psz.next()
                    em.op("pe", lambda e, pz=pz, kt=kt, qt=qt, kb=kb, t0=t0: e.matmul(
                        pz.t[:], kt.t[:, kb * 128:(kb + 1) * 128], qt.t[:, t0:t0 + 512], start=True, stop=True),
                        reads=[kt, qt], writes=[pz])
                    E = er.next()
                    em.op("act", lambda e, pz=pz, E=E: e.activation(out=E.t[:], in_=pz.t[:], func=AF.Exp),
                          reads=[pz], writes=[E])
                    SP = spr.next()
                    em.op("act", lambda e, SP=SP, E=E: e.activation(out=SP.t[:], in_=E.t[:], func=AF.Ln, bias=1.0),
                          reads=[E], writes=[SP])
                    if diag:
                        em.op("pool", lambda e, SP=SP, kb=kb, G=G: e.tensor_tensor(
                            SP.t[:], SP.t[:], mstr.t[:, kb - 4 * G, :], ALU.mult), reads=[SP, mstr], writes=[SP])
                    return SP

                def sb_back(h, G, kb, SP):
                    kt, qt, nq, v = hstate[h]
                    t0 = G * 512
                    nkb = 4 * (G + 1)
                    first = (kb == nkb - 1)
                    diag = kb >= 4 * G
                    pl = psl.next()
                    em.op("pe", lambda e, pl=pl, SP=SP: e.matmul(pl.t[:], tri.t[:], SP.t[:], start=True, stop=False),
                          reads=[tri, SP], writes=[pl])
                    if not first:
                        em.op("pe", lambda e, pl=pl: e.matmul(pl.t[:], ones.t[:], R.t[:], start=False, stop=False),
                              reads=[ones, R], writes=[pl])
                    em.op("pe", lambda e, pl=pl, kt=kt, nq=nq, kb=kb, t0=t0: e.matmul(
                        pl.t[:], kt.t[:, kb * 128:(kb + 1) * 128], nq.t[:, t0:t0 + 512], start=False, stop=True),
                        reads=[kt, nq], writes=[pl])
                    Wt = wr.next()
                    em.op("act", lambda e, pl=pl, Wt=Wt: e.activation(out=Wt.t[:], in_=pl.t[:], func=AF.Exp, scale=-1.0),
                          reads=[pl], writes=[Wt])
                    if diag:
                        em.op("pool", lambda e, Wt=Wt, kb=kb, G=G: e.tensor_tensor(
                            Wt.t[:], Wt.t[:], mstr.t[:, kb - 4 * G, :], ALU.mult), reads=[Wt, mstr], writes=[Wt])
                    em.op("pe", lambda e, Wt=Wt, v=v, kb=kb, first=first: e.matmul(
                        pso.t[0:64, :], v.t[:, kb, :], Wt.t[:], start=first, stop=(kb == 0)),
                        reads=[v, Wt], writes=[pso])
                    if kb > 0:
                        if first:
                            em.op("pool", lambda e, SP=SP: e.tensor_copy(R.t[:], SP.t[:]), reads=[SP], writes=[R])
                        else:
                            em.op("pool", lambda e, SP=SP: e.tensor_tensor(R.t[:], R.t[:], SP.t[:], ALU.add),
                                  reads=[SP, R], writes=[R])
                    else:
                        m = mo.next()
                        em.op("dve", lambda e, m=m: e.tensor_copy(m.t[:], pso.t[0:64, :]), reads=[pso], writes=[m])
                        em.dma("sp", mixT[M_SB + 64 * h:M_SB + 64 * h + 64, t0:t0 + 512], m.t[:], reads=[m])

                cur = sb_front(*items[0])
                for i, it in enumerate(items):
                    nxt = sb_front(*items[i + 1]) if i + 1 < len(items) else None
                    sb_back(*it, cur)
                    cur = nxt
                em.flush()

            if upto < 4:
                return nc, em.nins
            with ExitStack() as st:
                ikT = T(sb(st, "d_ikT", (64, S), BF16))
                dkT = T(sb(st, "d_dkT", (64, S), BF16))
                dv = T(sb(st, "d_dv", (128, NBLK, 66), BF16))
                isc = T(sb(st, "d_isc", (128, S), F32))
                msk = T(sb(st, "d_msk", (128, S), BF16))
                mT = T(sb(st, "d_mT", (128, NBLK, 256), BF16))
                iqr = Ring([sb(st, "d_iq%d" % i, (64, 8, 128), BF16) for i in range(2)])
                iwr = Ring([sb(st, "d_iw%d" % i, (128, 8), F32) for i in range(2)])
                dqr = Ring([sb(st, "d_dq%d" % i, (64, 5, 256), BF16) for i in range(2)])
                rr = Ring([sb(st, "d_r%d" % i, (128, 512), BF16) for i in range(3)])
                ptr_ = Ring([sb(st, "d_pt%d" % i, (128, 256), BF16) for i in range(3)])
                sm = T(sb(st, "d_sm", (128, 16), F32))
                wi = T(sb(st, "d_wi", (128, NIT), F32))
                osb = T(sb(st, "d_osb", (128, 256), F32))
                rb_ = T(sb(st, "d_rb", (64, 256), F32))
                mo = Ring([sb(st, "d_mo%d" % i, (64, 256), BF16) for i in range(2)])
                psi = Ring([ps(st, "d_psi%d" % i) for i in range(3)])
                pst = T(ps(st, "d_pst", (128, 512), BF16))
                pss_ = Ring([ps(st, "d_pss%d" % i, (128, 256)) for i in range(2)])
                pso = T(ps(st, "d_pso", (128, 256)))
                psb_ = T(ps(st, "d_psb", (128, 256)))
                em.dma("sp", ikT.t[:], projT[O_IK:O_IK + 64, :], writes=[ikT])
                em.dma("sp", dkT.t[:], projT[O_DK:O_DK + 64, :], writes=[dkT])
                em.dma("sp", dv.t[:], dsaV, writes=[dv])
                LO, MID, CNT, STEP, MX, MN, THR = 0, 1, 2, 3, 4, 5, 6
                for G2 in range(S // 256):
                    q0 = G2 * 256
                    dq = dqr.next()
                    em.dma("sp", dq.t[:], projT[O_DQ:O_DQ + 320, q0:q0 + 256].rearrange("(h p) t -> p h t", p=64), writes=[dq])
                    for qs in range(2):
                        QB = 2 * G2 + qs
                        qb0 = QB * 128
                        Lk = (QB + 1) * 128
                        iq, iw = iqr.next(), iwr.next()
                        em.dma("sp", iq.t[:], projT[O_IQ:O_IQ + 512, qb0:qb0 + 128].rearrange("(h p) t -> p h t", p=64), writes=[iq])
                        em.dma("sp", iw.t[:], idxW[QB], writes=[iw])
                        nch = (Lk + 511) // 512
                        for c_ in range(nch):
                            k0 = c_ * 512
                            wc = min(512, Lk - k0)
                            for h in range(8):
                                p = psi.next()
                                em.op("pe", lambda e, p=p, iq=iq, h=h, k0=k0, wc=wc: e.matmul(
                                    p.t[:, 0:wc], iq.t[:, h, :], ikT.t[:, k0:k0 + wc], start=True, stop=True),
                                    reads=[iq, ikT], writes=[p])
                                r = rr.next()
                                em.op("act", lambda e, p=p, r=r, wc=wc: e.activation(out=r.t[:, 0:wc], in_=p.t[:, 0:wc], func=AF.Relu),
                                      reads=[p], writes=[r])
                                if h == 0:
                                    em.op("dve", lambda e, r=r, iw=iw, k0=k0, wc=wc: e.tensor_scalar(
                                        isc.t[:, k0:k0 + wc], r.t[:, 0:wc], iw.t[:, 0:1], None, ALU.mult),
                                        reads=[r, iw], writes=[isc])
                                else:
                                    em.op("dve", lambda e, r=r, iw=iw, h=h, k0=k0, wc=wc: e.scalar_tensor_tensor(
                                        isc.t[:, k0:k0 + wc], r.t[:, 0:wc], iw.t[:, h:h + 1], isc.t[:, k0:k0 + wc], ALU.mult, ALU.add),
                                        reads=[r, iw, isc], writes=[isc])
                        em.op("dve", lambda e, qb0=qb0: e.tensor_tensor(
                            isc.t[:, qb0:qb0 + 128], isc.t[:, qb0:qb0 + 128], cadd.t[:], ALU.add), reads=[isc, cadd], writes=[isc])
                        if Lk <= TOPK:
                            em.op("dve", lambda e: e.memset(sm.t[:, THR:THR + 1], -1.0e29), writes=[sm])
                        else:
                            em.op("dve", lambda e, Lk=Lk: e.tensor_reduce(sm.t[:, MX:MX + 1], isc.t[:, 0:Lk], AX.X, ALU.max),
                                  reads=[isc], writes=[sm])
                            em.op("dve", lambda e, Lk=Lk: e.tensor_reduce(sm.t[:, LO:LO + 1], isc.t[:, 0:Lk - 128], AX.X, ALU.min),
                                  reads=[isc], writes=[sm])
                            em.op("dve", lambda e: e.tensor_tensor(sm.t[:, MN:MN + 1], sm.t[:, MX:MX + 1], sm.t[:, LO:LO + 1], ALU.subtract),
                                  reads=[sm], writes=[sm])
                            em.op("dve", lambda e: e.tensor_scalar(wi.t[:], pow2.t[:], sm.t[:, MN:MN + 1], None, ALU.mult),
                                  reads=[sm, pow2], writes=[wi])
                            for it in range(NIT):
                                em.op("dve", lambda e, it=it: e.tensor_tensor(
                                    sm.t[:, MID:MID + 1], sm.t[:, LO:LO + 1], wi.t[:, it:it + 1], ALU.add), reads=[sm, wi], writes=[sm])
                                em.op("dve", lambda e, Lk=Lk: e.tensor_scalar(
                                    msk.t[:, 0:Lk], isc.t[:, 0:Lk], sm.t[:, MID:MID + 1], 0.0, ALU.is_ge, ALU.add,
                                    accum_out=sm.t[:, CNT:CNT + 1]), reads=[isc, sm], writes=[msk, sm])
                                em.op("dve", lambda e, it=it: e.tensor_scalar(
                                    sm.t[:, STEP:STEP + 1], sm.t[:, CNT:CNT + 1], TOPK - 0.5, wi.t[:, it:it + 1], ALU.is_ge, ALU.mult),
                                    reads=[sm, wi], writes=[sm])
                                em.op("dve", lambda e: e.tensor_tensor(
                                    sm.t[:, LO:LO + 1], sm.t[:, LO:LO + 1], sm.t[:, STEP:STEP + 1], ALU.add), reads=[sm], writes=[sm])
                            em.op("dve", lambda e: e.tensor_copy(sm.t[:, THR:THR + 1], sm.t[:, LO:LO + 1]), reads=[sm], writes=[sm])
                        em.op("dve", lambda e, Lk=Lk: e.tensor_scalar(
                            msk.t[:, 0:Lk], isc.t[:, 0:Lk], sm.t[:, THR:THR + 1], None, ALU.is_ge), reads=[isc, sm], writes=[msk])
                        nb = QB + 1
                        for k4 in range(0, nb, 4):
                            n4 = min(4, nb - k4)
                            for i in range(n4):
                                em.op("pe", lambda e, k4=k4, i=i: e.transpose(
                                    pst.t[:, i * 128:(i + 1) * 128], msk.t[:, (k4 + i) * 128:(k4 + i + 1) * 128], ident.t[:]),
                                    reads=[msk, ident], writes=[pst])
                            em.op("act", lambda e, k4=k4, n4=n4, qs=qs: e.copy(
                                mT.t[:, k4:k4 + n4, qs * 128:(qs + 1) * 128],
                                pst.t[:, 0:n4 * 128].rearrange("p (a q) -> p a q", q=128)), reads=[pst], writes=[mT])
                        if qs == 0:
                            em.op("pool", lambda e, QB=QB: e.memset(mT.t[:, QB + 1, 0:128], 0.0), writes=[mT])
                    nkb = 2 * G2 + 2

                    def dsa_front(h, kb, dq=dq, G2=G2):
                        p = pss_.next()
                        em.op("pe", lambda e, p=p, dq=dq, h=h, kb=kb: e.matmul(
                            p.t[:, :], dkT.t[:, kb * 128:(kb + 1) * 128], dq.t[:, h, :], start=True, stop=True),
                            reads=[dkT, dq], writes=[p])
                        pt = ptr_.next()
                        em.op("act", lambda e, p=p, pt=pt, h=h: e.activation(
                            out=pt.t[:], in_=p.t[:], func=AF.Exp, bias=b31.t[:, h:h + 1]), reads=[p, b31], writes=[pt])
                        em.op("pool", lambda e, pt=pt, kb=kb: e.tensor_tensor(pt.t[:], pt.t[:], mT.t[:, kb, :], ALU.mult),
                              reads=[pt, mT], writes=[pt])
                        for qs in range(2):
                            dl = 2 * G2 + qs - kb
                            if dl in (0, 1):
                                em.op("pool", lambda e, pt=pt, qs=qs, dl=dl, h=h: e.tensor_tensor(
                                    pt.t[:, qs * 128:(qs + 1) * 128], pt.t[:, qs * 128:(qs + 1) * 128], ctab.t[:, dl, h, :], ALU.mult),
                                    reads=[pt, ctab], writes=[pt])
                        return pt

                    def dsa_back(h, kb, pt, nkb=nkb, q0=q0):
                        em.op("pe", lambda e, pt=pt, kb=kb, nkb=nkb: e.matmul(
                            pso.t[0:65, :], dv.t[:, kb, 0:65], pt.t[:], start=(kb == 0), stop=(kb == nkb - 1)),
                            reads=[dv, pt], writes=[pso])
                        if kb == nkb - 1:
                            em.op("act", lambda e: e.copy(osb.t[0:65, :], pso.t[0:65, :]), reads=[pso], writes=[osb])
                            em.op("pe", lambda e: e.matmul(psb_.t[0:64, :], sel.t[0:65, :], osb.t[0:65, :], start=True, stop=True),
                                  reads=[sel, osb], writes=[psb_])
                            em.op("dve", lambda e: e.reciprocal(rb_.t[:], psb_.t[0:64, :]), reads=[psb_], writes=[rb_])
                            m = mo.next()
                            em.op("dve", lambda e, m=m: e.tensor_tensor(m.t[:], osb.t[0:64, :], rb_.t[:], ALU.mult),
                                  reads=[osb, rb_], writes=[m])
                            em.dma("sp", mixT[M_DSA + 64 * h:M_DSA + 64 * h + 64, q0:q0 + 256], m.t[:], reads=[m])

                    ditems = [(h, kb) for h in range(5) for kb in range(nkb)]
                    cur = dsa_front(*ditems[0])
                    for i, it in enumerate(ditems):
                        nxt = dsa_front(*ditems[i + 1]) if i + 1 < len(ditems) else None
                        dsa_back(*it, cur)
                        cur = nxt
                em.flush()

            if upto < 5:
                return nc, em.nins
            with ExitStack() as st:
                wo = T(sb(st, "c_wo", (128, 8, D), BF16))
                mixr = Ring([sb(st, "c_mix%d" % i, (128, 8, 512), BF16) for i in range(2)])
                hinr = Ring([sb(st, "c_hin%d" % i, (128, 8, 512), F32) for i in range(2)])
                houtr = Ring([sb(st, "c_hout%d" % i, (128, 8, 512), F32) for i in range(2)])
                psr = Ring([ps(st, "c_ps%d" % i) for i in range(4)])
                wst = Ring([sb(st, "c_wst%d" % i, (128, WST), F32) for i in range(2)])
                wload(wo, W["w_o"][L], 8, wst)
                em.flush()
                for g in range(NG):
                    t0 = g * 512
                    mix, hin, hout = mixr.next(), hinr.next(), houtr.next()
                    em.dma("sp", mix.t[:], hview(mixT, t0), writes=[mix])
                    em.dma("sp", hin.t[:], hview(hsrc, t0), writes=[hin])
                    for fc in range(8):
                        p = psr.next()
                        for c_ in range(8):
                            em.op("pe", lambda e, p=p, c_=c_, fc=fc, mix=mix: e.matmul(
                                p.t[:], wo.t[:, c_, fc * 128:(fc + 1) * 128], mix.t[:, c_, :], start=(c_ == 0), stop=(c_ == 7)),
                                reads=[wo, mix], writes=[p])
                        em.op("dve", lambda e, p=p, fc=fc, hin=hin, hout=hout: e.tensor_tensor(
                            hout.t[:, fc, :], p.t[:], hin.t[:, fc, :], ALU.add), reads=[p, hin], writes=[hout])
                    em.dma("sp", hview(hT, t0), hout.t[:], reads=[hout])
                em.flush()

            if upto < 6:
                return nc, em.nins
            with ExitStack() as st:
                wg = T(sb(st, "f_wg", (128, 8, D_FF), BF16))
                wu = T(sb(st, "f_wu", (128, 8, D_FF), BF16))
                gf = T(sb(st, "f_gf", (128, 8), F32))
                hinr = Ring([sb(st, "f_hin%d" % i, (128, 8, 512), F32) for i in range(1)])
                sqt = T(sb(st, "f_sq", (128, 8, 512), BF16))
                hf = T(sb(st, "f_hf", (128, 8, 512), BF16))
                rs = T(sb(st, "f_rs", (128, 512), F32))
                sgr = Ring([sb(st, "f_sg%d" % i, (128, 512), F32) for i in range(2)])
                actr = Ring([sb(st, "f_act%d" % i, (128, NFF, 512), BF16) for i in range(1)])
                pss = T(ps(st, "f_pss"))
                psg = Ring([ps(st, "f_psg%d" % i) for i in range(3)])
                psu = Ring([ps(st, "f_psu%d" % i) for i in range(3)])
                wst = Ring([sb(st, "f_wst%d" % i, (128, WST), F32) for i in range(2)])
                wload(wg, W["w_gate"][L], 8, wst)
                wload(wu, W["w_up"][L], 8, wst)
                em.dma("sp", gf.t[:], W["g_ffn"][L], writes=[gf])
                em.flush()
                for g in range(NG):
                    t0 = g * 512
                    hin, at = hinr.next(), actr.next()
                    em.dma("sp", hin.t[:], hview(hT, t0), writes=[hin])
                    fm_norm(hin, gf, 8, sqt, pss, rs, 1.0 / D, EPS, hn=hf)
                    for f in range(NFF):
                        pg, pu = psg.next(), psu.next()
                        for (p, w_) in [(pg, wg), (pu, wu)]:
                            for c_ in range(8):
                                em.op("pe", lambda e, p=p, w_=w_, c_=c_, f=f: e.matmul(
                                    p.t[:], w_.t[:, c_, f * 128:(f + 1) * 128], hf.t[:, c_, :], start=(c_ == 0), stop=(c_ == 7)),
                                    reads=[w_, hf], writes=[p])
                        sg = sgr.next()
                        em.op("act", lambda e, pg=pg, sg=sg: e.activation(out=sg.t[:], in_=pg.t[:], func=AF.Silu), reads=[pg], writes=[sg])
                        em.op("dve", lambda e, pu=pu, sg=sg, at=at, f=f: e.tensor_tensor(at.t[:, f, :], sg.t[:], pu.t[:], ALU.mult),
                              reads=[pu, sg], writes=[at])
                    em.dma("sp", hview(actT, t0), at.t[:], reads=[at])
                em.flush()

            if upto < 7:
                return nc, em.nins
            with ExitStack() as st:
                last = (L == depth - 1)
                wd = T(sb(st, "g_wd", (128, NFF, D), BF16))
                wpg = T(sb(st, "g_wpg", (128, 8, D), BF16))
                wpp = T(sb(st, "g_wpp", (128, 2, D), BF16))
                gp = T(sb(st, "g_gp", (128, 8), F32))
                gfin = T(sb(st, "g_gfin", (128, 8), F32))
                atr = Ring([sb(st, "g_at%d" % i, (128, NFF, 512), BF16) for i in range(1)])
                hin = T(sb(st, "g_hin", (128, 8, 512), F32))
                h2 = T(sb(st, "g_h2", (128, 8, 512), F32))
                h3 = T(sb(st, "g_h3", (128, 8, 512), F32))
                sqt = T(sb(st, "g_sq", (128, 8, 512), BF16))
                hp = T(sb(st, "g_hp", (128, 8, 512), BF16))
                rs = T(sb(st, "g_rs", (128, 512), F32))
                ptl = Ring([sb(st, "g_pt%d" % i, (128, 2, 512), BF16) for i in range(1)])
                sgr = Ring([sb(st, "g_sg%d" % i, (128, 512), F32) for i in range(2)])
                pss = T(ps(st, "g_pss"))
                psr = Ring([ps(st, "g_ps%d" % i) for i in range(3)])
                psg = Ring([ps(st, "g_psg%d" % i) for i in range(2)])
                psp = Ring([ps(st, "g_psp%d" % i) for i in range(2)])
                wst = Ring([sb(st, "g_wst%d" % i, (128, 1024), F32) for i in range(2)])
                wload(wd, W["w_down"][L], NFF, wst)
                wload(wpg, W["w_ple_gate"][L], 8, wst)
                wload(wpp, W["w_ple_proj"][L], 2, wst)
                pstg = Ring([sb(st, "g_pstg%d" % i, (128, 2, 512), F32) for i in range(1)])
                em.dma("sp", gp.t[:], W["g_ple"][L], writes=[gp])
                em.dma("sp", gfin.t[:], W["g_fin"], writes=[gfin])
                em.flush()
                for g in range(NG):
                    t0 = g * 512
                    at, pt = atr.next(), ptl.next()
                    em.dma("sp", at.t[:], hview(actT, t0), writes=[at])
                    em.dma("sp", hin.t[:], hview(hT, t0), writes=[hin])
                    pq = pstg.next()
                    em.dma("sp", pq.t[:], pT[L].rearrange("(c p) t -> p c t", p=128)[:, :, t0:t0 + 512], writes=[pq])
                    em.op("pool", lambda e, pq=pq, pt=pt: e.tensor_copy(pt.t[:], pq.t[:]), reads=[pq], writes=[pt])
                    for fc in range(8):
                        p = psr.next()
                        for f in range(NFF):
                            em.op("pe", lambda e, p=p, f=f, fc=fc, at=at: e.matmul(
                                p.t[:], wd.t[:, f, fc * 128:(fc + 1) * 128], at.t[:, f, :], start=(f == 0), stop=(f == NFF - 1)),
                                reads=[wd, at], writes=[p])
                        em.op("dve", lambda e, p=p, fc=fc: e.tensor_tensor(h2.t[:, fc, :], p.t[:], hin.t[:, fc, :], ALU.add),
                              reads=[p, hin], writes=[h2])
                    fm_norm(h2, gp, 8, sqt, pss, rs, 1.0 / D, EPS, hn=hp)
                    for fc in range(8):
                        pg, pp_ = psg.next(), psp.next()
                        for c_ in range(8):
                            em.op("pe", lambda e, pg=pg, c_=c_, fc=fc: e.matmul(
                                pg.t[:], wpg.t[:, c_, fc * 128:(fc + 1) * 128], hp.t[:, c_, :], start=(c_ == 0), stop=(c_ == 7)),
                                reads=[wpg, hp], writes=[pg])
                        for j in range(2):
                            em.op("pe", lambda e, pp_=pp_, j=j, fc=fc, pt=pt: e.matmul(
                                pp_.t[:], wpp.t[:, j, fc * 128:(fc + 1) * 128], pt.t[:, j, :], start=(j == 0), stop=(j == 1)),
                                reads=[wpp, pt], writes=[pp_])
                        sg = sgr.next()
                        em.op("act", lambda e, pg=pg, sg=sg: e.activation(out=sg.t[:], in_=pg.t[:], func=AF.Sigmoid), reads=[pg], writes=[sg])
                        em.op("dve", lambda e, pp_=pp_, sg=sg: e.tensor_tensor(sg.t[:], sg.t[:], pp_.t[:], ALU.mult),
                              reads=[pp_, sg], writes=[sg])
                        em.op("pool", lambda e, sg=sg, fc=fc: e.tensor_tensor(h3.t[:, fc, :], sg.t[:], h2.t[:, fc, :], ALU.add),
                              reads=[sg, h2], writes=[h3])
                    if not last:
                        em.dma("sp", hview(hT, t0), h3.t[:], reads=[h3])
                    else:
                        fm_norm(h3, gfin, 8, sqt, pss, rs, 1.0 / D, EPS, hn=None)
                        for c_ in range(8):
                            em.op("dve", lambda e, c_=c_: e.scalar_tensor_tensor(
                                h2.t[:, c_, :], h3.t[:, c_, :], gfin.t[:, c_:c_ + 1], rs.t[:], ALU.mult, ALU.mult),
                                reads=[h3, gfin, rs], writes=[h2])
                        em.dma("sp", hview(yT, t0), h2.t[:], reads=[h2])
                em.flush()
    return nc, em.nins


def run(inp, S, depth, nb, debug=False, upto=99):
    w = prep_weights({k: np.asarray(v, np.float32) for k, v in inp.items() if k not in ("x", "p")})
    consts = make_consts(S)
    nc, nins = build(S, depth, debug, upto)
    x = np.asarray(inp["x"], np.float32)
    p = np.asarray(inp["p"], np.float32)
    in_maps = []
    for b in range(nb):
        m = {"xT": np.ascontiguousarray(x[b].T), "pT": np.ascontiguousarray(p[:, b].transpose(0, 2, 1))}
        m.update(w)
        for k, v in consts.items():
            m["c_" + k] = v
        in_maps.append(m)
    res = run_bass_kernel_spmd(nc, in_maps, core_ids=list(range(nb)))
    out = np.stack([np.ascontiguousarray(res.results[b]["yT"].T) for b in range(nb)], 0).astype(np.float32)
    return out, res


def kernel(**inputs):
    out, _ = run(inputs, SEQ, DEPTH, BATCH)
    return out
```
